# Optimizing a Trainium2 kernel written in Bass

```python
import jax
import jax.numpy as jnp
from jax import lax
import numpy as np

D_MODEL = 2048
BATCH = 2
SEQ = 4096
DEPTH = 1

HEAD_DIM = 128
FOX_HEADS = 6
DSA_HEADS = 6
CROSS_HEADS = 4
N_MEM = 256
FOX_WIDTH = FOX_HEADS * HEAD_DIM
DSA_WIDTH = DSA_HEADS * HEAD_DIM
CROSS_WIDTH = CROSS_HEADS * HEAD_DIM
N_BRANCHES = 3
ROPE_THETA = 500000.0
ROT_DIM = HEAD_DIM // 4
IDX_HEADS = 16
IDX_DIM = 64
IDX_ROT_DIM = IDX_DIM // 4
INDEX_TOPK = 256
Q_BLOCK = 128
N_GROUPS = 4
EXPERTS_PER_GROUP = 8
N_EXPERTS = N_GROUPS * EXPERTS_PER_GROUP
MOE_TOP_K = 2
EXPERT_FF = 1024
MOE_BLOCK = 128
EPS = 1e-6
IN_SPLITS = (FOX_WIDTH, FOX_WIDTH, FOX_WIDTH, FOX_HEADS,
             DSA_WIDTH, DSA_WIDTH, DSA_WIDTH,
             IDX_HEADS * IDX_DIM, IDX_HEADS, IDX_DIM,
             CROSS_WIDTH, N_BRANCHES * D_MODEL)
IN_WIDTH = sum(IN_SPLITS)

kernel_name = 'hybrid_fox_dsa_xattn_hmoe'


def rms_norm(x, g):
    xf = x.astype(jnp.float32)
    y = xf * lax.rsqrt(jnp.mean(xf * xf, axis=-1, keepdims=True) + EPS)
    return (y * g.astype(jnp.float32)).astype(x.dtype)


def partial_rope(x, positions, rot_dim):
    half = rot_dim // 2
    inv_freq = jnp.power(jnp.float32(ROPE_THETA), -jnp.arange(half, dtype=jnp.float32) * 2.0 / rot_dim)
    ang = positions.astype(jnp.float32)[..., None] * inv_freq
    cos = jnp.cos(ang)[:, :, None, :]
    sin = jnp.sin(ang)[:, :, None, :]
    xf = x.astype(jnp.float32)
    x1, x2, rest = xf[..., :half], xf[..., half:rot_dim], xf[..., rot_dim:]
    out = jnp.concatenate([x1 * cos - x2 * sin, x2 * cos + x1 * sin, rest], axis=-1)
    return out.astype(x.dtype)


def to_blocks(a):
    b, s = a.shape[:2]
    return jnp.moveaxis(a.reshape((b, s // Q_BLOCK, Q_BLOCK) + a.shape[2:]), 1, 0)


def from_blocks(a):
    a = jnp.moveaxis(a, 0, 1)
    return a.reshape((a.shape[0], a.shape[1] * a.shape[2]) + a.shape[3:])


def fox_attention(q, k, v, log_f):
    s_len, dh = q.shape[1], q.shape[3]
    cum = jnp.cumsum(log_f.astype(jnp.float32), axis=1)
    cum_k = jnp.moveaxis(cum, 1, 2)
    kpos = jnp.arange(s_len)
    qpos = kpos.reshape(-1, Q_BLOCK)
    scale = dh ** -0.5

    def block(args):
        qb, cqb, qp = args
        s = jnp.einsum('bqhd,bkhd->bhqk', qb, k, preferred_element_type=jnp.float32) * scale
        s = s + jnp.moveaxis(cqb, 1, 2)[..., None] - cum_k[:, :, None, :]
        s = jnp.where(kpos[None, :] <= qp[:, None], s, -jnp.inf)
        p = jax.nn.softmax(s, axis=-1)
        return jnp.einsum('bhqk,bkhd->bqhd', p.astype(v.dtype), v)

    return from_blocks(lax.map(block, (to_blocks(q), to_blocks(cum), qpos)))


def dsa_attention(q, k, v, q_idx, k_idx, w_idx):
    s_len, dh = q.shape[1], q.shape[3]
    topk = min(INDEX_TOPK, s_len // 4)
    kpos = jnp.arange(s_len)
    qpos = kpos.reshape(-1, Q_BLOCK)
    scale = dh ** -0.5
    gather = jax.vmap(lambda kb, ib: kb[ib])

    def block(args):
        qb, qib, wb, qp = args
        logits = jnp.einsum('bqjd,bkd->bqjk', qib, k_idx, preferred_element_type=jnp.float32) * IDX_DIM ** -0.5
        score = jnp.einsum('bqj,bqjk->bqk', wb.astype(jnp.float32), jax.nn.relu(logits))
        score = jnp.where((kpos[None, :] <= qp[:, None])[None], score, -jnp.inf)
        _, idx = lax.top_k(score, topk)
        valid = idx <= qp[None, :, None]
        kg = gather(k, idx)
        vg = gather(v, idx)
        s = jnp.einsum('bqhd,bqkhd->bhqk', qb, kg, preferred_element_type=jnp.float32) * scale
        s = jnp.where(valid[:, None], s, -jnp.inf)
        p = jax.nn.softmax(s, axis=-1)
        return jnp.einsum('bhqk,bqkhd->bqhd', p.astype(vg.dtype), vg)

    blocks = (to_blocks(q), to_blocks(q_idx), to_blocks(w_idx), qpos)
    return from_blocks(lax.map(block, blocks))


def cross_attention(q, k_mem, v_mem):
    s = jnp.einsum('bqhd,bmhd->bhqm', q, k_mem, preferred_element_type=jnp.float32) * q.shape[-1] ** -0.5
    p = jax.nn.softmax(s, axis=-1)
    return jnp.einsum('bhqm,bmhd->bqhd', p.astype(v_mem.dtype), v_mem)


def hier_moe(h, wg, bg, we, be, w_gate, w_up, w_down):
    b, s, d = h.shape
    n = b * s
    hf = h.reshape(n, d)
    g_logits = jnp.einsum('nd,dg->ng', hf, wg, preferred_element_type=jnp.float32) + bg.astype(jnp.float32)
    p_group = jax.nn.softmax(g_logits, axis=-1)
    grp = jnp.argmax(g_logits, axis=-1).astype(jnp.int32)
    p_sel = jnp.take_along_axis(p_group, grp[:, None], axis=1)
    e_logits = jnp.einsum('nd,de->ne', hf, we, preferred_element_type=jnp.float32) + be.astype(jnp.float32)
    e_in = jnp.take_along_axis(e_logits.reshape(n, N_GROUPS, EXPERTS_PER_GROUP), grp[:, None, None], axis=1)[:, 0]
    top_l, top_j = lax.top_k(e_in, MOE_TOP_K)
    gate = p_sel * jax.nn.softmax(top_l, axis=-1)
    eid = (grp[:, None] * EXPERTS_PER_GROUP + top_j).reshape(-1).astype(jnp.int32)
    tok = jnp.repeat(jnp.arange(n, dtype=jnp.int32), MOE_TOP_K)
    gw = gate.reshape(-1)
    a = n * MOE_TOP_K
    n_slots = -(-(a + N_EXPERTS * (MOE_BLOCK - 1)) // MOE_BLOCK) * MOE_BLOCK
    n_blocks = n_slots // MOE_BLOCK
    counts = jnp.zeros((N_EXPERTS,), jnp.int32).at[eid].add(1)
    padded = (counts + MOE_BLOCK - 1) // MOE_BLOCK * MOE_BLOCK
    pad_end = jnp.cumsum(padded)
    pad_start = pad_end - padded
    seg_start = jnp.cumsum(counts) - counts
    order = jnp.argsort(eid, stable=True)
    se = eid[order]
    dest = pad_start[se] + jnp.arange(a, dtype=jnp.int32) - seg_start[se]
    slot_tok = jnp.full((n_slots,), n, jnp.int32).at[dest].set(tok[order])
    slot_gate = jnp.zeros((n_slots,), jnp.float32).at[dest].set(gw[order])
    block_e = jnp.minimum(jnp.searchsorted(pad_end, jnp.arange(n_blocks, dtype=jnp.int32) * MOE_BLOCK, side='right'), N_EXPERTS - 1)
    h_pad = jnp.concatenate([hf, jnp.zeros((1, d), hf.dtype)], axis=0)
    xs = h_pad[slot_tok].reshape(n_blocks, MOE_BLOCK, d)

    def expert_block(args):
        xb, e = args
        hid = jax.nn.silu(xb @ w_gate[e]) * (xb @ w_up[e])
        return hid @ w_down[e]

    ys = lax.map(expert_block, (xs, block_e)).reshape(n_slots, d)
    out = jax.ops.segment_sum(ys * slot_gate[:, None].astype(ys.dtype), slot_tok, num_segments=n + 1)
    return out[:n].reshape(b, s, d)


def hybrid_layer(x, mem, positions, attn_norm_g, mem_norm_g, w_in, fox_forget_b,
                 fox_q_norm_g, fox_k_norm_g, dsa_q_norm_g, dsa_k_norm_g, idx_k_norm_g,
                 cross_q_norm_g, cross_k_norm_g, w_mem_kv, w_branch_fox, w_branch_dsa,
                 w_branch_cross, w_out, ffn_norm_g, router_group_w, router_group_b,
                 router_expert_w, router_expert_b, expert_w_gate, expert_w_up, expert_w_down):
    b, s, _ = x.shape
    h = rms_norm(x, attn_norm_g)
    proj = jnp.einsum('bsd,de->bse', h, w_in)
    offsets = [int(o) for o in np.cumsum(IN_SPLITS)[:-1]]
    fq, fk, fv, f_logit, dq, dk, dv, iq, iw, ik, cq, gates = jnp.split(proj, offsets, axis=-1)

    def heads(t, n_heads, dim):
        return t.reshape(t.shape[0], t.shape[1], n_heads, dim)

    fq = rms_norm(heads(fq, FOX_HEADS, HEAD_DIM), fox_q_norm_g)
    fk = rms_norm(heads(fk, FOX_HEADS, HEAD_DIM), fox_k_norm_g)
    log_f = jax.nn.log_sigmoid(f_logit.astype(jnp.float32) + fox_forget_b.astype(jnp.float32))
    o_fox = fox_attention(fq, fk, heads(fv, FOX_HEADS, HEAD_DIM), log_f).reshape(b, s, FOX_WIDTH)

    dq = partial_rope(rms_norm(heads(dq, DSA_HEADS, HEAD_DIM), dsa_q_norm_g), positions, ROT_DIM)
    dk = partial_rope(rms_norm(heads(dk, DSA_HEADS, HEAD_DIM), dsa_k_norm_g), positions, ROT_DIM)
    iq = partial_rope(heads(iq, IDX_HEADS, IDX_DIM), positions, IDX_ROT_DIM)
    ik = partial_rope(rms_norm(ik, idx_k_norm_g)[:, :, None, :], positions, IDX_ROT_DIM)[:, :, 0]
    iw = iw * IDX_HEADS ** -0.5
    o_dsa = dsa_attention(dq, dk, heads(dv, DSA_HEADS, HEAD_DIM), iq, ik, iw).reshape(b, s, DSA_WIDTH)

    m = rms_norm(mem, mem_norm_g)
    mk, mv = jnp.split(jnp.einsum('bmd,de->bme', m, w_mem_kv), 2, axis=-1)
    mk = rms_norm(heads(mk, CROSS_HEADS, HEAD_DIM), cross_k_norm_g)
    cq = rms_norm(heads(cq, CROSS_HEADS, HEAD_DIM), cross_q_norm_g)
    o_cross = cross_attention(cq, mk, heads(mv, CROSS_HEADS, HEAD_DIM)).reshape(b, s, CROSS_WIDTH)

    g_fox, g_dsa, g_cross = jnp.split(jax.nn.sigmoid(gates), N_BRANCHES, axis=-1)
    merged = (g_fox * (o_fox @ w_branch_fox)
              + g_dsa * (o_dsa @ w_branch_dsa)
              + g_cross * (o_cross @ w_branch_cross))
    x = x + merged @ w_out

    x = x + hier_moe(rms_norm(x, ffn_norm_g), router_group_w, router_group_b, router_expert_w,
                     router_expert_b, expert_w_gate, expert_w_up, expert_w_down)
    return x


def setup_inputs(seed: int = 0) -> dict:
    key = jax.random.key(seed)
    ks = jax.random.split(key, 32)
    f32 = jnp.float32

    def w(k, shape, fan_in):
        return jax.random.normal(k, shape, f32) * fan_in ** -0.5

    def gain(k, shape):
        return 1.0 + 0.05 * jax.random.normal(k, shape, f32)

    L, D = DEPTH, D_MODEL
    offset = jax.random.randint(ks[2], (BATCH, 1), 0, 1024, dtype=jnp.int32)
    return {
        'x': jax.random.normal(ks[0], (BATCH, SEQ, D), f32),
        'mem': jax.random.normal(ks[1], (BATCH, N_MEM, D), f32),
        'positions': offset + jnp.arange(SEQ, dtype=jnp.int32)[None, :],
        'attn_norm_g': gain(ks[3], (L, D)),
        'mem_norm_g': gain(ks[4], (L, D)),
        'w_in': w(ks[5], (L, D, IN_WIDTH), D),
        'fox_forget_b': jax.random.uniform(ks[6], (L, FOX_HEADS), f32, 1.0, 6.0),
        'fox_q_norm_g': gain(ks[7], (L, HEAD_DIM)),
        'fox_k_norm_g': gain(ks[8], (L, HEAD_DIM)),
        'dsa_q_norm_g': gain(ks[9], (L, HEAD_DIM)),
        'dsa_k_norm_g': gain(ks[10], (L, HEAD_DIM)),
        'idx_k_norm_g': gain(ks[11], (L, IDX_DIM)),
        'cross_q_norm_g': gain(ks[12], (L, HEAD_DIM)),
        'cross_k_norm_g': gain(ks[13], (L, HEAD_DIM)),
        'w_mem_kv': w(ks[14], (L, D, 2 * CROSS_WIDTH), D),
        'w_branch_fox': w(ks[15], (L, FOX_WIDTH, D), FOX_WIDTH),
        'w_branch_dsa': w(ks[16], (L, DSA_WIDTH, D), DSA_WIDTH),
        'w_branch_cross': w(ks[17], (L, CROSS_WIDTH, D), CROSS_WIDTH),
        'w_out': w(ks[18], (L, D, D), D),
        'ffn_norm_g': gain(ks[19], (L, D)),
        'router_group_w': w(ks[20], (L, D, N_GROUPS), D),
        'router_group_b': 0.01 * jax.random.normal(ks[21], (L, N_GROUPS), f32),
        'router_expert_w': w(ks[22], (L, D, N_EXPERTS), D),
        'router_expert_b': 0.01 * jax.random.normal(ks[23], (L, N_EXPERTS), f32),
        'expert_w_gate': w(ks[24], (L, N_EXPERTS, D, EXPERT_FF), D),
        'expert_w_up': w(ks[25], (L, N_EXPERTS, D, EXPERT_FF), D),
        'expert_w_down': w(ks[26], (L, N_EXPERTS, EXPERT_FF, D), EXPERT_FF),
    }


def reference(x, mem, positions, attn_norm_g, mem_norm_g, w_in, fox_forget_b,
              fox_q_norm_g, fox_k_norm_g, dsa_q_norm_g, dsa_k_norm_g, idx_k_norm_g,
              cross_q_norm_g, cross_k_norm_g, w_mem_kv, w_branch_fox, w_branch_dsa,
              w_branch_cross, w_out, ffn_norm_g, router_group_w, router_group_b,
              router_expert_w, router_expert_b, expert_w_gate, expert_w_up, expert_w_down):
    for layer in range(DEPTH):
        x = hybrid_layer(
            x, mem, positions, attn_norm_g[layer], mem_norm_g[layer], w_in[layer],
            fox_forget_b[layer], fox_q_norm_g[layer], fox_k_norm_g[layer],
            dsa_q_norm_g[layer], dsa_k_norm_g[layer], idx_k_norm_g[layer],
            cross_q_norm_g[layer], cross_k_norm_g[layer], w_mem_kv[layer],
            w_branch_fox[layer], w_branch_dsa[layer], w_branch_cross[layer], w_out[layer],
            ffn_norm_g[layer], router_group_w[layer], router_group_b[layer],
            router_expert_w[layer], router_expert_b[layer], expert_w_gate[layer],
            expert_w_up[layer], expert_w_down[layer])
    return x
```

```python
import numpy as np
from contextlib import ExitStack
import concourse.bass as bass
import concourse.mybir as mybir
from concourse.bass_utils import run_bass_kernel_spmd

F32 = mybir.dt.float32
BF16 = mybir.dt.bfloat16
I32 = mybir.dt.int32
U32 = mybir.dt.uint32
AF = mybir.ActivationFunctionType
ALU = mybir.AluOpType
AX = mybir.AxisListType


class Trk:
    __slots__ = ("w", "r")

    def __init__(self):
        self.w = None
        self.r = {}


class Buf:
    def __init__(self, h):
        self.h = h
        self.t = Trk()
        self.parts = {}

    def __getitem__(self, key):
        return self.h[key]

    def part(self, i):
        p = self.parts.get(i)
        if p is None:
            p = self.parts[i] = Trk()
        return p


def _trk(x):
    return x.t if isinstance(x, Buf) else x


class Eng:
    def __init__(self, name, q, selfsync):
        self.name = name
        self.q = q
        self.selfsync = selfsync
        self.sem = None
        self.semkey = None
        self.cnt = 0
        self.seen = {}


class KB:
    SEM_EPOCH = 20000
    NDMA = 8

    def __init__(self, nc, es):
        self.nc = nc
        self.es = es
        self.sems = {}
        self.nsem = 0
        self.engs = {
            "pe": Eng("pe", nc.tensor, False),
            "act": Eng("act", nc.scalar, True),
            "dve": Eng("dve", nc.vector, True),
            "pool": Eng("pool", nc.gpsimd, True),
            "sp": Eng("sp", nc.sync, False),
        }
        for e in self.engs.values():
            self._new_sem(e)
        self.dma = {}
        for qn in ("sp", "pool", "act"):
            ring = []
            for i in range(self.NDMA):
                key = self._alloc_sem(f"d{qn}{i}")
                ring.append([key, 0])
            self.dma[qn] = [ring, 0]
        self.nins = 0

    def _alloc_sem(self, name):
        s = self.es.enter_context(self.nc.semaphore(f"{name}_{self.nsem}"))
        key = self.nsem
        self.nsem += 1
        self.sems[key] = s
        return key

    def _new_sem(self, e):
        e.semkey = self._alloc_sem(e.name)
        e.sem = self.sems[e.semkey]
        e.cnt = 0

    def sb(self, name, shape, dtype):
        h = self.es.enter_context(self.nc.sbuf_tensor("s_" + name, list(shape), dtype))
        return Buf(h)

    def ps(self, name, shape, dtype=F32):
        h = self.es.enter_context(self.nc.psum_tensor("p_" + name, list(shape), dtype))
        return Buf(h)

    def _waits(self, e, reads, writes):
        need = {}

        def add(ev, same_ok):
            if ev is None:
                return
            k, v = ev
            if k == e.semkey and not same_ok:
                return
            if e.seen.get(k, 0) >= v:
                return
            if need.get(k, 0) < v:
                need[k] = v

        for t in reads:
            t = _trk(t)
            add(t.w, e.selfsync)
        for t in writes:
            t = _trk(t)
            add(t.w, e.selfsync)
            for k, v in t.r.items():
                add((k, v), False)
        for k, v in need.items():
            e.q.wait_ge(self.sems[k], v)
            e.seen[k] = v

    def _mark(self, ev, reads, writes):
        k, v = ev
        for t in reads:
            t = _trk(t)
            if t.r.get(k, 0) < v:
                t.r[k] = v
        for t in writes:
            t = _trk(t)
            t.w = ev
            t.r = {}

    def op(self, eng, fn, reads=(), writes=()):
        e = self.engs[eng]
        self._waits(e, reads, writes)
        ins = fn(e.q)
        e.cnt += 1
        ins.then_inc(e.sem, 1)
        ev = (e.semkey, e.cnt)
        self._mark(ev, reads, writes)
        if e.cnt >= self.SEM_EPOCH:
            self._new_sem(e)
        self.nins += 1
        return ins

    def dmaq(self, qn, out, in_, reads=(), writes=(), **kw):
        e = self.engs[qn]
        self._waits(e, reads, writes)
        ring, pos = self.dma[qn]
        slot = ring[pos % self.NDMA]
        self.dma[qn][1] = pos + 1
        key, val = slot
        if val > 0 and e.seen.get(key, 0) < val:
            e.q.wait_ge(self.sems[key], val)
            e.seen[key] = val
        ins = e.q.dma_start(out=out, in_=in_, **kw)
        val += 16
        slot[1] = val
        ins.then_inc(self.sems[key], 16)
        ev = (key, val)
        self._mark(ev, reads, writes)
        self.nins += 1
        return ev

    def wait_all(self, eng, trks):
        e = self.engs[eng]
        self._waits(e, trks, trks)

    def mm(self, out, lhsT, rhs, start, stop, reads, writes, **kw):
        return self.op("pe", lambda q: q.matmul(out, lhsT, rhs, start=start, stop=stop, **kw), reads, writes)

    def tr(self, out, in_, ident, reads, writes):
        return self.op("pe", lambda q: q.transpose(out, in_, ident), reads, writes)

    def act(self, out, in_, func, reads, writes, eng="act", **kw):
        return self.op(eng, lambda q: q.activation(out=out, in_=in_, func=func, **kw), reads, writes)

    def tt(self, eng, out, in0, in1, op, reads, writes):
        return self.op(eng, lambda q: q.tensor_tensor(out=out, in0=in0, in1=in1, op=op), reads, writes)

    def ts(self, eng, out, in0, s1, s2, op0, op1, reads, writes, **kw):
        if op1 is None:
            return self.op(eng, lambda q: q.tensor_scalar(out=out, in0=in0, scalar1=s1, scalar2=None, op0=op0, **kw), reads, writes)
        return self.op(eng, lambda q: q.tensor_scalar(out=out, in0=in0, scalar1=s1, scalar2=s2, op0=op0, op1=op1, **kw), reads, writes)

    def stt(self, out, in0, scalar, in1, op0, op1, reads, writes):
        return self.op("dve", lambda q: q.scalar_tensor_tensor(out=out, in0=in0, scalar=scalar, in1=in1, op0=op0, op1=op1), reads, writes)

    def cp(self, eng, out, in_, reads, writes):
        if eng == "act":
            return self.op(eng, lambda q: q.copy(out=out, in_=in_), reads, writes)
        return self.op(eng, lambda q: q.tensor_copy(out=out, in_=in_), reads, writes)


D = 2048
KC = D // 128
DH = 128
FH = 6
CH = 4
NMEM = 256
IH = 16
IDD = 64
FF = 1024
EPS = 1e-6
TOPK = 256
NEG = -30000.0
TWO_PI = 2.0 * np.pi

K_FK, K_DK, K_FV, K_DV, K_IK, K_FL = 0, 768, 1536, 2304, 3072, 3136
CK = 3142
Q_FQ, Q_DQ, Q_CQ, Q_IQ, Q_IW, Q_GT = 0, 768, 1536, 2048, 3072, 3088
CQ = 3088 + 3 * D


class Cfg:
    def __init__(self, S=4096, G=4, EPG=8):
        self.S = S
        self.NB = S // 128
        self.NQ = self.NB // 4
        self.T = self.NQ * 128
        self.G = G
        self.EPG = EPG
        self.E = G * EPG
        self.topk = min(TOPK, S // 4)


def build_nc(cfg, debug=False):
    S, NB, NQ, T, E, G, EPG = cfg.S, cfg.NB, cfg.NQ, cfg.T, cfg.E, cfg.G, cfg.EPG
    NR = G + E
    nc = bass.Bass("TRN2", target_bir_lowering=False)

    def din(name, shape, dt=F32):
        return nc.dram_tensor(name, list(shape), dt, kind="ExternalInput").ap()

    xb = din("xb", [S, D])
    xo = din("xo", [T, D])
    memb = din("memb", [NMEM, D])
    posb2 = din("posb2", [128, NB], I32)
    poso2 = din("poso2", [128, NQ], I32)
    wk = din("wk", [D, CK])
    wq = din("wq", [D, CQ])
    wmem = din("wmem", [D, 2 * CH * DH])
    wbf_d = din("wbf", [FH * DH, D])
    wbd_d = din("wbd", [FH * DH, D])
    wbc_d = din("wbc", [CH * DH, D])
    wout_d = din("wout", [D, D])
    wr_d = din("wr", [D, NR])
    wg_d = din("wg", [E, D, FF])
    wu_d = din("wu", [E, D, FF])
    wd_d = din("wd", [E, FF, D])
    gcols_d = din("gcols", [128, 3 * KC])
    gh_d = din("gh", [6 * 128 + 64 + 6 + NR + 24])
    ident_d = din("ident", [128, 128])
    cm_d = din("cm", [128, 4, 128])
    cmT_d = din("cmT", [128, 4, 128])
    sel_d = din("sel", [128, 4, 128])
    out_d = nc.dram_tensor("out", [T, D], F32, kind="ExternalOutput").ap()
    dbg = {}
    if debug:
        for nm, shp in debug.items():
            dbg[nm] = nc.dram_tensor("dbg_" + nm, list(shp), F32, kind="ExternalOutput").ap()

    ksc = nc.dram_tensor("ksc", [S, CK], F32).ap()
    qsc = nc.dram_tensor("qsc", [T, CQ], F32).ap()
    msc = nc.dram_tensor("msc", [NMEM, 2 * CH * DH], F32).ap()
    x1sc = nc.dram_tensor("x1sc", [T, D], F32).ap()
    t_ksc, t_qsc, t_msc, t_x1 = Trk(), Trk(), Trk(), Trk()

    with ExitStack() as es:
        k = KB(nc, es)
        PB = [k.ps(f"pb{i}", [128, 512], F32) for i in range(8)]

        def pbf(i):
            return PB[i][:].bitcast(BF16)

        identF = k.sb("identF", [128, 128], F32)
        identB = k.sb("identB", [128, 128], BF16)
        onesB = k.sb("onesB", [128, 128], BF16)
        onesF = k.sb("onesF", [128, 128], F32)
        tri = k.sb("tri", [128, 128], F32)
        cmF = k.sb("cmF", [128, 4, 128], F32)
        cmB = k.sb("cmB", [128, 4, 128], BF16)
        cmT = k.sb("cmTs", [128, 4, 128], F32)
        selF = k.sb("selF", [128, 4, 128], F32)
        gcols = k.sb("gcols", [128, 3 * KC], F32)
        NGH = 6 * 128 + 64 + 6 + NR + 24
        gh = k.sb("gh", [128, NGH], F32)
        k.dmaq("sp", identF[:], ident_d[:, :], writes=[identF])
        k.dmaq("sp", cmF[:], cm_d[:, :, :], writes=[cmF])
        k.dmaq("sp", cmT[:], cmT_d[:, :, :], writes=[cmT])
        k.dmaq("sp", selF[:], sel_d[:, :, :], writes=[selF])
        k.dmaq("sp", gcols[:], gcols_d[:, :], writes=[gcols])
        k.dmaq("sp", gh[:], gh_d.partition_broadcast(128), writes=[gh])
        k.cp("dve", identB[:], identF[:], [identF], [identB])
        k.cp("dve", cmB[:], cmF[:], [cmF], [cmB])
        k.op("pool", lambda q: q.memset(onesB[:], 1.0), [], [onesB])
        k.op("pool", lambda q: q.memset(onesF[:], 1.0), [], [onesF])
        k.op("pool", lambda q: q.memset(tri[:], 1.0), [], [tri])
        k.op("pool", lambda q: q.affine_select(out=tri[:], in_=tri[:], pattern=[[1, 128]], compare_op=ALU.is_ge,
                                               fill=0.0, base=0, channel_multiplier=-1), [tri], [tri])
        G_FQ, G_FK, G_DQ, G_DK, G_CQ, G_CK = [gh[:, i * 128:(i + 1) * 128] for i in range(6)]
        G_IK = gh[:, 768:832]
        FB = gh[:, 832:838]
        RB = gh[:, 838:838 + NR]
        IFR = gh[:, 838 + NR:838 + NR + 24]
        gA, gM, gFf = 0, KC, 2 * KC

        small = {}

        def barrier():
            evs = []
            for e in k.engs.values():
                if e.cnt > 0:
                    evs.append((e.semkey, e.cnt))
            for qn in k.dma:
                for key, val in k.dma[qn][0]:
                    if val > 0:
                        evs.append((key, val))
            for e in k.engs.values():
                for key, val in evs:
                    if key == e.semkey:
                        continue
                    if e.seen.get(key, 0) < val:
                        e.q.wait_ge(k.sems[key], val)
                        e.seen[key] = val

        def scast(eng, out, in_, sc_ap, reads, writes):
            if eng == "act":
                k.act(out, in_, AF.Identity, reads, writes, scale=sc_ap)
            elif eng == "pool":
                k.ts("pool", out, in_, sc_ap, 1.0, ALU.mult, ALU.mult, reads, writes)
            else:
                k.ts("dve", out, in_, sc_ap, None, ALU.mult, None, reads, writes)

        def rstd_from_ss(ss, n, inv_n):
            k.ts("dve", ss, ss, inv_n, EPS, ALU.mult, ALU.add, [small["ss"]], [small["ss"]])
            k.act(ss, ss, AF.Sqrt, [small["ss"]], [small["ss"]])
            k.op("dve", lambda q: q.reciprocal(out=ss, in_=ss), [small["ss"]], [small["ss"]])

        def head_norm(srcbuf, src, H, dd, gain, dstbuf, dst, sqbuf):
            ssb = small["ss"]
            ss = ssb[:, 0:H]
            k.tt("dve", sqbuf[:, 0:H * dd], src, src, ALU.mult, [srcbuf], [sqbuf])
            k.op("dve", lambda q: q.tensor_reduce(out=ss, in_=sqbuf[:, 0:H * dd].rearrange("p (h d) -> p h d", h=H),
                                                  axis=AX.X, op=ALU.add), [sqbuf], [ssb])
            rstd_from_ss(ss, H, 1.0 / dd)
            s3 = src.rearrange("p (h d) -> p h d", h=H)
            d3 = dst.rearrange("p (h d) -> p h d", h=H)
            k.tt("dve", d3, s3, ss.unsqueeze(2).to_broadcast([128, H, dd]), ALU.mult, [srcbuf, ssb], [dstbuf])
            k.tt("dve", d3, d3, gain.unsqueeze(1).to_broadcast([128, H, dd]), ALU.mult, [dstbuf], [dstbuf])

        def sincos(pos_ap_dram, csbuf):
            pi_ = small["posi"]
            pf = small["posf"]
            a = small["ang"]
            ki = small["angi"]
            kf = small["angf"]
            m = small["angm"]
            k.dmaq("sp", pi_[:], pos_ap_dram, writes=[pi_])
            k.cp("dve", pf[:], pi_[:], [pi_], [pf])
            k.ts("dve", a[:], IFR, pf[:, 0:1], None, ALU.mult, None, [pf, gh], [a])
            k.cp("dve", ki[:], a[:], [a], [ki])
            k.cp("dve", kf[:], ki[:], [ki], [kf])
            k.tt("dve", a[:], a[:], kf[:], ALU.subtract, [a, kf], [a])

            def wrap(dst, src, shift, rd):
                if shift != 0.0:
                    k.ts("dve", dst, src, shift, None, ALU.add, None, rd, [csbuf])
                    src = dst
                    rd = [csbuf]
                k.ts("dve", m[:], src, 0.5, None, ALU.is_ge, None, rd, [m])
                k.tt("dve", dst, src, m[:], ALU.subtract, rd + [m], [csbuf])
                k.ts("dve", m[:], dst, -0.5, None, ALU.is_lt, None, [csbuf], [m])
                k.tt("dve", dst, dst, m[:], ALU.add, [csbuf, m], [csbuf])
            sn = csbuf[:, 24:48]
            cs = csbuf[:, 0:24]
            wrap(sn, a[:], 0.0, [a])
            wrap(cs, sn, 0.25, [csbuf])
            k.act(csbuf[:, 0:48], csbuf[:, 0:48], AF.Sin, [csbuf], [csbuf], scale=TWO_PI)

        def rope(buf, view3, H, half, cos, sin, tmpbuf):
            x1 = view3[:, :, 0:half]
            x2 = view3[:, :, half:2 * half]
            cb = cos.unsqueeze(1).to_broadcast([128, H, half])
            sb_ = sin.unsqueeze(1).to_broadcast([128, H, half])
            t = tmpbuf[:, 0:4 * H * half].rearrange("p (a h d) -> p a h d", a=4, h=H)
            k.tt("dve", t[:, 0], x1, cb, ALU.mult, [buf, small["cs"]], [tmpbuf])
            k.tt("dve", t[:, 1], x2, sb_, ALU.mult, [buf, small["cs"]], [tmpbuf])
            k.tt("dve", t[:, 2], x2, cb, ALU.mult, [buf, small["cs"]], [tmpbuf])
            k.tt("dve", t[:, 3], x1, sb_, ALU.mult, [buf, small["cs"]], [tmpbuf])
            k.tt("dve", x1, t[:, 0], t[:, 1], ALU.subtract, [tmpbuf], [buf])
            k.tt("dve", x2, t[:, 2], t[:, 3], ALU.add, [tmpbuf], [buf])

        with ExitStack() as es1:
            k.es = es1
            small["ss"] = k.sb("ss1", [128, 16], F32)
            TG = min(1024, S)
            hT = k.sb("hT", [128, KC, TG], BF16)
            xin = [k.sb(f"xin{i}", [128, D], F32) for i in range(2)]
            hb = [k.sb(f"hb{i}", [128, D], BF16) for i in range(2)]
            wst = [k.sb(f"wst{i}", [128, KC, 512], F32) for i in range(1)]
            wbfb = [k.sb(f"wbfb{i}", [128, KC, 512], BF16) for i in range(2)]
            osb = [k.sb(f"osb{i}", [128, 512], F32) for i in range(3)]
            ssx = [k.sb(f"ssx{i}", [128, 1], F32) for i in range(2)]
            cnt = {"x": 0, "w": 0, "o": 0, "p": 0}

            def make_hT(src_dram, nblk):
                for tb in range(nblk):
                    i = cnt["x"] % 2
                    cnt["x"] += 1
                    xi, hbi, ssi = xin[i], hb[i], ssx[i]
                    k.dmaq("sp", xi[:], src_dram[tb * 128:(tb + 1) * 128, :], writes=[xi])
                    k.act(hbi[:], xi[:], AF.Square, [xi], [hbi, ssi], accum_out=ssi[:])
                    k.ts("dve", ssi[:], ssi[:], 1.0 / D, EPS, ALU.mult, ALU.add, [ssi], [ssi])
                    k.act(ssi[:], ssi[:], AF.Sqrt, [ssi], [ssi])
                    k.op("dve", lambda q: q.reciprocal(out=ssi[:], in_=ssi[:]), [ssi], [ssi])
                    k.ts("dve", hbi[:], xi[:], ssi[:, 0:1], None, ALU.mult, None, [xi, ssi], [hbi])
                    for half in range(2):
                        pb = PB[cnt["p"] % 2]
                        cnt["p"] += 1
                        pv = pb[:].bitcast(BF16).rearrange("p (c t) -> p c t", c=8)
                        for c in range(8):
                            kc = half * 8 + c
                            k.tr(pv[:, c, :], hbi[:, kc * 128:(kc + 1) * 128], identB[:], [hbi, identB], [pb])
                        eng = "act" if half == 0 else "dve"
                        k.cp(eng, hT[:, half * 8:(half + 1) * 8, tb * 128:(tb + 1) * 128], pv, [pb], [hT.part(tb)])

            def proj(nblk, W, C, gofs, dst, dst_trk):
                chunks = list(range(0, C, 512))
                Wv = W.rearrange("(kc p) c -> p kc c", p=128)
                ws = wst[0]

                def issue_load(c0):
                    cw = min(512, C - c0)
                    k.dmaq("sp", ws[:, :, 0:cw], Wv[:, :, c0:c0 + cw], writes=[ws])
                issue_load(chunks[0])
                for ci, c0 in enumerate(chunks):
                    cw = min(512, C - c0)
                    wb = wbfb[cnt["w"] % 2]
                    cnt["w"] += 1
                    for kc in range(KC):
                        eng = ("dve", "act")[kc % 2]
                        scast(eng, wb[:, kc, 0:cw], ws[:, kc, 0:cw], gcols[:, gofs + kc:gofs + kc + 1], [ws, gcols], [wb.part(kc)])
                    if ci + 1 < len(chunks):
                        issue_load(chunks[ci + 1])
                    for tb in range(nblk):
                        pb = PB[2 + cnt["o"] % 3]
                        ob = osb[cnt["o"] % 3]
                        cnt["o"] += 1
                        for kc in range(KC):
                            k.mm(pb[:, 0:cw], hT[:, kc, tb * 128:(tb + 1) * 128], wb[:, kc, 0:cw], kc == 0, kc == KC - 1,
                                 [hT.part(tb), wb.part(kc)], [pb])
                        k.cp("act", ob[:, 0:cw], pb[:, 0:cw], [pb], [ob])
                        k.dmaq("sp", dst[tb * 128:(tb + 1) * 128, c0:c0 + cw], ob[:, 0:cw], reads=[ob], writes=[dst_trk])

            for grp in range(S // TG):
                make_hT(xb[grp * TG:(grp + 1) * TG, :], TG // 128)
                proj(TG // 128, wk, CK, gA, ksc[grp * TG:(grp + 1) * TG, :], t_ksc)
            make_hT(xo, NQ)
            proj(NQ, wq, CQ, gA, qsc, t_qsc)
            make_hT(memb, NMEM // 128)
            proj(NMEM // 128, wmem, 2 * CH * DH, gM, msc, t_msc)
            barrier()
        k.es = es
        OT_guard = nc.sbuf_tensor("OT", [128, FH + FH + CH, T], BF16, side="right")
        OT = Buf(OT_guard.__enter__())
        mbsc = nc.dram_tensor("mbsc", [NQ, 128, NB * 128], BF16).ap()
        t_mbsc = Trk()

        with ExitStack() as es2:
            k.es = es2
            small["ss"] = k.sb("ss2", [128, 16], F32)
            CSA = k.sb("CSA", [128, NB, 48], F32)
            CSO = k.sb("CSO", [128, NQ, 48], F32)
            with ExitStack() as est:
                k.es = est
                for (pos2, nblk, dst) in ((posb2, NB, CSA), (poso2, NQ, CSO)):
                    pi_ = k.sb(f"pi{nblk}", [128, nblk], I32)
                    pf = k.sb(f"pf{nblk}", [128, nblk], F32)
                    a = k.sb(f"an{nblk}", [128, nblk, 24], F32)
                    ki = k.sb(f"ki{nblk}", [128, nblk, 24], I32)
                    kf = k.sb(f"kf{nblk}", [128, nblk, 24], F32)
                    m = k.sb(f"mm{nblk}", [128, nblk, 24], F32)
                    k.dmaq("sp", pi_[:], pos2[:, :], writes=[pi_])
                    k.cp("dve", pf[:], pi_[:], [pi_], [pf])
                    k.tt("dve", a[:], pf[:].unsqueeze(2).to_broadcast([128, nblk, 24]),
                         IFR.unsqueeze(1).to_broadcast([128, nblk, 24]), ALU.mult, [pf, gh], [a])
                    k.cp("dve", ki[:], a[:], [a], [ki])
                    k.cp("dve", kf[:], ki[:], [ki], [kf])
                    k.tt("dve", a[:], a[:], kf[:], ALU.subtract, [a, kf], [a])
                    sn = dst[:, :, 24:48]
                    cs = dst[:, :, 0:24]
                    k.ts("dve", m[:], a[:], 0.5, None, ALU.is_ge, None, [a], [m])
                    k.tt("dve", sn, a[:], m[:], ALU.subtract, [a, m], [dst])
                    k.ts("dve", m[:], sn, -0.5, None, ALU.is_lt, None, [dst], [m])
                    k.tt("dve", sn, sn, m[:], ALU.add, [dst, m], [dst])
                    k.ts("dve", cs, sn, 0.25, None, ALU.add, None, [dst], [dst])
                    k.ts("dve", m[:], cs, 0.5, None, ALU.is_ge, None, [dst], [m])
                    k.tt("dve", cs, cs, m[:], ALU.subtract, [dst, m], [dst])
                    k.ts("dve", m[:], cs, -0.5, None, ALU.is_lt, None, [dst], [m])
                    k.tt("dve", cs, cs, m[:], ALU.add, [dst, m], [dst])
                    k.act(dst[:], dst[:], AF.Sin, [dst], [dst], scale=TWO_PI)
                barrier()
            k.es = es2

            def csv(tab, b_):
                small["cs"] = tab
                return tab[:, b_, 0:16], tab[:, b_, 16:24], tab[:, b_, 24:40], tab[:, b_, 40:48]
            NCK = k.sb("NCK", [128, NB, FH], F32)
            carry = k.sb("carry", [128, FH], F32)
            kin = [k.sb(f"kin{i}", [128, 1544], F32) for i in range(2)]
            kn2 = [k.sb(f"kn{i}", [128, 1024], F32) for i in range(2)]
            sqb2 = [k.sb(f"sqb{i}", [128, 768], F32) for i in range(2)]
            knb2 = [k.sb(f"knb{i}", [128, 1024], BF16) for i in range(2)]
            rtmp2 = [k.sb(f"rtmp{i}", [128, 512], F32) for i in range(2)]
            ss2 = [k.sb(f"ssp{i}", [128, 16], F32) for i in range(2)]
            lf2 = [k.sb(f"lfp{i}", [128, 8], F32) for i in range(2)]
            kn, sqb, knb, rtmp = kn2[0], sqb2[0], knb2[0], rtmp2[0]
            lf = k.sb("lf", [128, 8], F32)
            qT = k.sb("qT", [128, FH, 128], BF16)
            pT_sb = [k.sb(f"pTsb{i}", [128, 128], BF16) for i in range(4)]
            rec = [k.sb(f"rec{i}", [128, 128], F32) for i in range(2)]
            cref = k.sb("cref", [128, FH], F32)
            biasall = k.sb("biasall", [128, NB, FH], F32)
            pcnt = {"s": 0, "t": 0, "k": 0, "a": 0}
            qin = kin[0]
            qn, qnb = kn, knb

            def tr_bank():
                pb = PB[0]
                return pb, pb[:].bitcast(BF16).rearrange("p (c t) -> p c t", c=8)

            def kside_block(mode, tb, KT, VV):
                par = tb % 2
                ki_, knp, sqp, knbp, rtp, ssp, lfp = kin[par], kn2[par], sqb2[par], knb2[par], rtmp2[par], ss2[par], lf2[par]
                pb = PB[0] if par == 0 else PB[1]
                pv = pb[:].bitcast(BF16).rearrange("p (c t) -> p c t", c=8)
                rows = slice(tb * 128, (tb + 1) * 128)
                kcol = K_FK if mode == "fox" else K_DK
                vcol = K_FV if mode == "fox" else K_DV
                k.dmaq("sp", ki_[:, 0:768], ksc[rows, kcol:kcol + 768], reads=[t_ksc], writes=[ki_])
                k.dmaq("sp", ki_[:, 768:1536], ksc[rows, vcol:vcol + 768], reads=[t_ksc], writes=[ki_])
                if mode == "fox":
                    k.dmaq("sp", ki_[:, 1536:1542], ksc[rows, K_FL:K_FL + 6], reads=[t_ksc], writes=[ki_])
                yield
                src = ki_[:, 0:768]
                ss = ssp[:, 0:FH]
                k.tt("dve", sqp[:, 0:768], src, src, ALU.mult, [ki_], [sqp])
                k.op("dve", lambda q: q.tensor_reduce(out=ss, in_=sqp[:, 0:768].rearrange("p (h d) -> p h d", h=FH),
                                                      axis=AX.X, op=ALU.add), [sqp], [ssp])
                k.ts("dve", ss, ss, 1.0 / DH, EPS, ALU.mult, ALU.add, [ssp], [ssp])
                if mode == "fox":
                    k.tt("dve", lfp[:, 0:6], ki_[:, 1536:1542], FB, ALU.add, [ki_, gh], [lfp])
                yield
                k.act(ss, ss, AF.Sqrt, [ssp], [ssp])
                if mode == "fox":
                    k.act(lfp[:, 0:6], lfp[:, 0:6], AF.Exp, [lfp], [lfp], scale=-1.0)
                    k.act(lfp[:, 0:6], lfp[:, 0:6], AF.Ln, [lfp], [lfp], bias=1.0)
                yield
                k.op("dve", lambda q: q.reciprocal(out=ss, in_=ss), [ssp], [ssp])
                s3 = src.rearrange("p (h d) -> p h d", h=FH)
                d3 = knp[:, 0:768].rearrange("p (h d) -> p h d", h=FH)
                gain = G_FK if mode == "fox" else G_DK
                k.tt("dve", d3, s3, ss.unsqueeze(2).to_broadcast([128, FH, DH]), ALU.mult, [ki_, ssp], [knp])
                k.tt("dve", d3, d3, gain.unsqueeze(1).to_broadcast([128, FH, DH]), ALU.mult, [knp], [knp])
                if mode == "dsaB":
                    c16, c8, s16, s8 = csv(CSA, tb)
                    rope(knp, d3, FH, 16, c16, s16, rtp)
                k.cp("pool", VV[:, tb, :], ki_[:, 768:1536], [ki_], [VV.part(tb)])
                yield
                k.cp("act", knbp[:, 0:768], knp[:, 0:768], [knp], [knbp])
                yield
                for h in range(FH):
                    k.tr(pv[:, h, :], knbp[:, h * 128:(h + 1) * 128], identB[:], [knbp, identB], [pb])
                if mode == "fox":
                    pc = PB[7]
                    k.mm(pc[:, par * 32:par * 32 + 6], tri[:], lfp[:, 0:6], True, True, [tri, lfp], [pc])
                    k.mm(pc[:, par * 32 + 8:par * 32 + 14], onesF[:], lfp[:, 0:6], True, True, [onesF, lfp], [pc])
                yield
                k.cp("dve", KT[:, :, rows], pv[:, 0:FH, :], [pb], [KT.part(tb)])
                if mode == "fox":
                    pc = PB[7]
                    k.tt("dve", NCK[:, tb, :], pc[:, par * 32:par * 32 + 6], carry[:], ALU.add, [pc, carry], [NCK.part(tb)])
                    k.tt("dve", carry[:], pc[:, par * 32 + 8:par * 32 + 14], carry[:], ALU.add, [pc, carry], [carry])

            def kside_pass(mode, KT, VV, IKT):
                if mode == "fox":
                    k.op("pool", lambda q: q.memset(carry[:], 0.0), [], [carry])
                if mode != "dsaA":
                    for tb0 in range(0, NB, 2):
                        gens = [kside_block(mode, tb0, KT, VV), kside_block(mode, tb0 + 1, KT, VV)]
                        live = list(gens)
                        while live:
                            nxt = []
                            for g_ in live:
                                try:
                                    next(g_)
                                    nxt.append(g_)
                                except StopIteration:
                                    pass
                            live = nxt
                    return
                for tb in range(NB):
                    ki_ = kin[pcnt["k"] % 2]
                    pcnt["k"] += 1
                    rows = slice(tb * 128, (tb + 1) * 128)
                    COS16, COS8, SIN16, SIN8 = csv(CSA, tb)
                    k.dmaq("sp", ki_[:, 0:64], ksc[rows, K_IK:K_IK + 64], reads=[t_ksc], writes=[ki_])
                    head_norm(ki_, ki_[:, 0:64], 1, 64, G_IK, kn, kn[:, 0:64], sqb)
                    rope(kn, kn[:, 0:64].rearrange("p (h d) -> p h d", h=1), 1, 8, COS8, SIN8, rtmp)
                    k.cp("dve", kn[:, 64:128], kn[:, 0:64], [kn], [kn])
                    k.cp("act", knb[:, 0:128], kn[:, 0:128], [kn], [knb])
                    pb, pv = tr_bank()
                    k.tr(pv[:, 0, :], knb[:, 0:128], identB[:], [knb, identB], [pb])
                    k.cp("dve", IKT[0][0:64, rows], pv[0:64, 0, :], [pb], [IKT[0].part(tb)])
                    k.cp("dve", IKT[1][64:128, rows], pv[64:128, 0, :], [pb], [IKT[1].part(tb)])

            def attention(i, nheads, nkb, KT, VV, mask_fn, bias_fn, ot_base):
                scale = DH ** -0.5
                for h in range(nheads):
                    pO, pD = ((PB[5], PB[6]), (PB[7], PB[1]))[pcnt["a"] % 2]
                    pcnt["a"] += 1
                    pend = []

                    def pv_step(kb, pt):
                        kr = [KT.part(kb), VV.part(kb)]
                        k.mm(pO[:, 0:128], VV[:, kb, h * 128:(h + 1) * 128], pt[:], kb == 0, kb == nkb - 1, kr + [pt], [pO])
                        k.mm(pD[:, 0:128], onesB[:], pt[:], kb == 0, kb == nkb - 1, [onesB, pt], [pD])
                    for kb in range(nkb):
                        pS = PB[2 + pcnt["s"] % 3]
                        pt = pT_sb[pcnt["s"] % 4]
                        pcnt["s"] += 1
                        mk = mask_fn(kb) if mask_fn else None
                        kr = [KT.part(kb), VV.part(kb)]
                        k.mm(pS[:, 0:128], KT[:, h, kb * 128:(kb + 1) * 128], qT[:, h, :], True, mk is None, kr + [qT], [pS])
                        if mk is not None:
                            k.mm(pS[:, 0:128], identB[:], mk[0], False, True, [identB] + mk[1], [pS])
                        if bias_fn is not None:
                            bap, brd = bias_fn(kb, h)
                            k.act(pt[:], pS[:, 0:128], AF.Exp, [pS] + brd, [pt], scale=scale, bias=bap)
                        else:
                            k.act(pt[:], pS[:, 0:128], AF.Exp, [pS], [pt], scale=scale)
                        pend.append((kb, pt))
                        if len(pend) > 2:
                            pv_step(*pend.pop(0))
                    while pend:
                        pv_step(*pend.pop(0))
                    rc = rec[pcnt["a"] % 2]
                    k.op("dve", lambda q: q.reciprocal(out=rc[:], in_=pD[:, 0:128]), [pD], [rc])
                    k.tt("dve", OT[:, ot_base + h, i * 128:(i + 1) * 128], pO[:, 0:128], rc[:], ALU.mult, [pO, rc],
                         [OT.part((ot_base + h, i))])

            def load_q_heads(i, col, H, gain, do_rope, cs16=None):
                rows = slice(i * 128, (i + 1) * 128)
                k.dmaq("sp", qin[:, 0:H * DH], qsc[rows, col:col + H * DH], reads=[t_qsc], writes=[qin])
                head_norm(qin, qin[:, 0:H * DH], H, DH, gain, qn, qn[:, 0:H * DH], sqb)
                if do_rope:
                    rope(qn, qn[:, 0:H * DH].rearrange("p (h d) -> p h d", h=H), H, 16, cs16[0], cs16[1], rtmp)
                k.cp("act", qnb[:, 0:H * DH], qn[:, 0:H * DH], [qn], [qnb])
                pb, pv = tr_bank()
                for h in range(H):
                    k.tr(pv[:, h, :], qnb[:, h * 128:(h + 1) * 128], identB[:], [qnb, identB], [pb])
                k.cp("dve", qT[:, 0:H, :], pv[:, 0:H, :], [pb], [qT])

            def dump_ot(name, h0, h1):
                if debug and name in dbg:
                    tmpd = kn
                    for h in range(h0, h1):
                        k.cp("dve", tmpd[:, 0:T], OT[:, h, :], [OT.part((h, i_)) for i_ in range(NQ)], [tmpd])
                        k.dmaq("sp", dbg[name][(h - h0) * 128:(h - h0 + 1) * 128, :], tmpd[:, 0:T], reads=[tmpd])

            with ExitStack() as esf:
                k.es = esf
                KT = k.sb("KTf", [128, FH, S], BF16)
                VV = k.sb("VVf", [128, NB, FH * DH], BF16)
                kside_pass("fox", KT, VV, None)
                for i in range(NQ):
                    nkb = 4 * (i + 1)
                    load_q_heads(i, Q_FQ, FH, G_FQ, False)
                    pc = PB[7]
                    for r in range(4):
                        k.mm(pc[:, 16:22], selF[:, r, :], NCK[:, 4 * i + r, :], r == 0, r == 3, [selF, NCK.part(4 * i + r)], [pc])
                    k.cp("dve", cref[:], pc[:, 16:22], [pc], [cref])
                    for kb in range(nkb):
                        k.tt("dve", biasall[:, kb, :], NCK[:, kb, :], cref[:], ALU.subtract, [NCK.part(kb), cref], [biasall])
                    attention(i, FH, nkb, KT, VV,
                              (lambda kb, i=i: ((cmB[:, kb - 4 * i, :], [cmB]) if kb >= 4 * i else None)),
                              (lambda kb, h: (biasall[:, kb, h:h + 1], [biasall])), 0)
                dump_ot("ofox", 0, FH)
                barrier()
            k.es = es2

            with ExitStack() as esa:
                k.es = esa
                IKT0 = k.sb("IKT0", [128, S], BF16)
                IKT1 = k.sb("IKT1", [128, S], BF16)
                SCb = [k.sb(f"SC{i}", [128, S], F32) for i in range(2)]
                WK = k.sb("WK", [128, 512], F32)
                MB = k.sb("MB", [128, S], BF16)
                MBT = k.sb("MBT", [128, NB, 128], BF16)
                mx = k.sb("mx", [128, 8], F32)
                bs = k.sb("bs", [128, 8], F32)
                NIT = 16
                HV = k.sb("HV", [128, NIT], F32)
                pw2 = k.sb("pw2", [128, NIT], F32)
                for it in range(NIT):
                    k.op("pool", lambda q, it=it: q.memset(pw2[:, it:it + 1], 2.0 ** -(it + 1)), [], [pw2])
                rl = [k.sb(f"rl{i}", [128, 512], F32) for i in range(5)]
                iqT = k.sb("iqT", [128, 8, 128], BF16)
                wsc = k.sb("wsc", [128, IH], F32)
                Dg = k.sb("Dg", [128, IH, 128], F32)
                k.op("pool", lambda q: q.memset(IKT0[:], 0.0), [], [IKT0])
                k.op("pool", lambda q: q.memset(IKT1[:], 0.0), [], [IKT1])
                kside_pass("dsaA", None, None, (IKT0, IKT1))
                IK = (IKT0, IKT1)
                icnt = 0
                acnt = 0
                cmTf = cmT[:].rearrange("p r k -> p (r k)")
                for i in range(NQ):
                    nkb = 4 * (i + 1)
                    L = nkb * 128
                    SC = SCb[i % 2]
                    rows = slice(i * 128, (i + 1) * 128)
                    COS16, COS8, SIN16, SIN8 = csv(CSO, i)
                    k.dmaq("sp", qin[:, 0:1024 + 16], qsc[rows, Q_IQ:Q_IQ + 1024 + 16], reads=[t_qsc], writes=[qin])
                    rope(qin, qin[:, 0:1024].rearrange("p (h d) -> p h d", h=IH), IH, 8, COS8, SIN8, rtmp)
                    k.cp("act", qnb[:, 0:1024], qin[:, 0:1024], [qin], [qnb])
                    k.ts("dve", wsc[:], qin[:, 1024:1040], (IH ** -0.5) * (IDD ** -0.5), None, ALU.mult, None, [qin], [wsc])
                    for j in range(IH):
                        k.ts("pool", Dg[:, j, :], identF[:], wsc[:, j:j + 1], 1.0, ALU.mult, ALU.mult, [identF, wsc], [Dg])
                    pb, pv = tr_bank()
                    for m in range(8):
                        k.tr(pv[:, m, :], qnb[:, m * 128:(m + 1) * 128], identB[:], [qnb, identB], [pb])
                    k.cp("dve", iqT[:], pv, [pb], [iqT])
                    nch = L // 512
                    for kc in range(nch):
                        ks = slice(kc * 512, (kc + 1) * 512)
                        pA = PB[5 + acnt % 2]
                        acnt += 1
                        last = (kc == nch - 1)
                        pend = []

                        def diag(jj, rbuf):
                            k.mm(pA[:], Dg[:, jj, :], rbuf[:], jj == 0, (jj == IH - 1) and not last, [Dg, rbuf], [pA])
                        for j in range(IH):
                            m, r = j // 2, j % 2
                            pS = PB[1 + icnt % 4]
                            rb_ = rl[icnt % 5]
                            icnt += 1
                            k.mm(pS[:], iqT[:, m, :], IK[r][:, ks], True, True,
                                 [iqT] + [IK[r].part(kc * 4 + t_) for t_ in range(4)], [pS])
                            k.act(rb_[:], pS[:], AF.Relu, [pS], [rb_])
                            pend.append((j, rb_))
                            if len(pend) > 3:
                                diag(*pend.pop(0))
                        while pend:
                            diag(*pend.pop(0))
                        if last:
                            k.mm(pA[:], identF[:], cmTf, False, True, [identF, cmT], [pA])
                        k.cp("act", SC[:, ks], pA[:], [pA], [SC.part(kc)])
                    scp = [SC.part(c) for c in range(nch)]
                    if i == 0:
                        cur, curp = SC, scp
                        nround = cfg.topk // 8
                        for rnd in range(nround):
                            k.op("dve", lambda q, cur=cur: q.max(out=mx[:], in_=cur[:, 0:L]), curp, [mx])
                            if rnd < nround - 1:
                                k.op("dve", lambda q, cur=cur: q.match_replace(out=WK[:, 0:L], in_to_replace=mx[:],
                                                                              in_values=cur[:, 0:L], imm_value=-1e30),
                                     curp + [mx], [WK])
                                cur, curp = WK, [WK]
                        k.ts("dve", mx[:, 7:8], mx[:, 7:8], -10000.0, None, ALU.max, None, [mx], [mx])
                    else:
                        lo, hi, mid, cntc, ge = (bs[:, c:c + 1] for c in range(5))
                        k.op("dve", lambda q: q.tensor_reduce(out=lo, in_=SC[:, 0:L - 512], axis=AX.X, op=ALU.min), scp, [bs])
                        k.op("dve", lambda q: q.tensor_reduce(out=hi, in_=SC[:, 0:L], axis=AX.X, op=ALU.max), scp, [bs])
                        k.ts("dve", hi, hi, 1e-3, None, ALU.add, None, [bs], [bs])
                        k.tt("dve", hi, hi, lo, ALU.subtract, [bs], [bs])
                        k.ts("dve", HV[:], pw2[:], hi, None, ALU.mult, None, [pw2, bs], [HV])
                        for it in range(NIT):
                            k.tt("dve", mid, lo, HV[:, it:it + 1], ALU.add, [bs, HV], [bs])
                            k.op("dve", lambda q: q.tensor_scalar(out=MB[:, 0:L], in0=SC[:, 0:L], scalar1=mid, scalar2=None,
                                                                  op0=ALU.is_ge, op1=ALU.add, accum_out=cntc),
                                 scp + [bs], [MB, bs])
                            k.ts("dve", ge, cntc, float(cfg.topk) - 0.5, None, ALU.is_ge, None, [bs], [bs])
                            k.stt(lo, ge, HV[:, it:it + 1], lo, ALU.mult, ALU.add, [bs, HV], [bs])
                        k.cp("dve", mx[:, 7:8], lo, [bs], [mx])
                    k.ts("dve", MB[:, 0:L], SC[:, 0:L], mx[:, 7:8], NEG, ALU.is_lt, ALU.mult, scp + [mx], [MB])
                    for kb0 in range(0, nkb, 4):
                        pb, pv = tr_bank()
                        for c in range(4):
                            kb = kb0 + c
                            k.tr(pv[:, c, :], MB[:, kb * 128:(kb + 1) * 128], identB[:], [MB, identB], [pb])
                        k.cp("act", MBT[:, kb0:kb0 + 4, :], pv[:, 0:4, :], [pb], [MBT])
                    k.dmaq("pool", mbsc[i, :, 0:L], MBT[:, 0:nkb, :].rearrange("p a b -> p (a b)"), reads=[MBT], writes=[t_mbsc])
                barrier()
            k.es = es2

            with ExitStack() as esb:
                k.es = esb
                KT = k.sb("KTd", [128, FH, S], BF16)
                VV = k.sb("VVd", [128, NB, FH * DH], BF16)
                MBI = [k.sb(f"MBI{i}", [128, NB, 128], BF16) for i in range(1)]
                kside_pass("dsaB", KT, VV, None)
                for i in range(NQ):
                    nkb = 4 * (i + 1)
                    L = nkb * 128
                    rows = slice(i * 128, (i + 1) * 128)
                    mbi = MBI[0]
                    k.dmaq("sp", mbi[:, 0:nkb, :].rearrange("p a b -> p (a b)"), mbsc[i, :, 0:L], reads=[t_mbsc], writes=[mbi])
                    COS16, COS8, SIN16, SIN8 = csv(CSO, i)
                    load_q_heads(i, Q_DQ, FH, G_DQ, True, (COS16, SIN16))
                    attention(i, FH, nkb, KT, VV, (lambda kb, mbi=mbi: (mbi[:, kb, :], [mbi])), None, FH)
                dump_ot("odsa", FH, 2 * FH)
                barrier()
                for mb_ in range(NMEM // 128):
                    ki_ = kin[1]
                    rows = slice(mb_ * 128, (mb_ + 1) * 128)
                    k.dmaq("sp", ki_[:, 0:1024], msc[rows, :], reads=[t_msc], writes=[ki_])
                    head_norm(ki_, ki_[:, 0:512], CH, DH, G_CK, kn, kn[:, 0:512], sqb)
                    k.cp("act", knb[:, 0:512], kn[:, 0:512], [kn], [knb])
                    pb, pv = tr_bank()
                    for h in range(CH):
                        k.tr(pv[:, h, :], knb[:, h * 128:(h + 1) * 128], identB[:], [knb, identB], [pb])
                    k.cp("dve", KT[:, 0:CH, rows], pv[:, 0:CH, :], [pb], [KT.part(mb_)])
                    k.cp("pool", VV[:, mb_, 0:512], ki_[:, 512:1024], [ki_], [VV.part(mb_)])
                for i in range(NQ):
                    load_q_heads(i, Q_CQ, CH, G_CQ, False)
                    attention(i, CH, NMEM // 128, KT, VV, None, None, 2 * FH)
                dump_ot("ocross", 2 * FH, 2 * FH + CH)
                barrier()
            k.es = es2
            barrier()
        k.es = es

        with ExitStack() as es3:
            k.es = es3
            small["ss"] = k.sb("ss3", [128, 16], F32)
            MT = k.sb("MT", [128, KC, T], BF16)
            stg = [k.sb(f"stg{i}", [128, 1024], F32) for i in range(2)]
            sc_ = 0
            pcn = 0
            with ExitStack() as es3a:
                k.es = es3a
                WB = k.sb("WB", [128, 2 * FH + CH, D], BF16)
                for bi, (wsrc, nh) in enumerate(((wbf_d, FH), (wbd_d, FH), (wbc_d, CH))):
                    for h in range(nh):
                        hh = (0, FH, 2 * FH)[bi] + h
                        for hf in range(2):
                            st = stg[sc_ % 2]
                            sc_ += 1
                            k.dmaq("sp", st[:], wsrc[h * 128:(h + 1) * 128, hf * 1024:(hf + 1) * 1024], writes=[st])
                            k.cp("dve" if sc_ % 2 else "act", WB[:, hh, hf * 1024:(hf + 1) * 1024], st[:], [st], [WB.part(hh)])
                gt = k.sb("gt", [128, 3 * D], F32)
                mg = k.sb("mg", [128, D], F32)
                mgt = k.sb("mgt", [128, 512], F32)
                mgb = k.sb("mgb", [128, D], BF16)
                for i in range(NQ):
                    rows = slice(i * 128, (i + 1) * 128)
                    k.dmaq("sp", gt[:], qsc[rows, Q_GT:Q_GT + 3 * D], reads=[t_qsc], writes=[gt])
                    k.act(gt[:], gt[:], AF.Sigmoid, [gt], [gt])
                    for bi, (nh, base) in enumerate(((FH, 0), (FH, FH), (CH, 2 * FH))):
                        for dc in range(4):
                            ds_ = slice(dc * 512, (dc + 1) * 512)
                            pb = PB[2 + pcn % 3]
                            pcn += 1
                            for h in range(nh):
                                k.mm(pb[:], OT[:, base + h, rows], WB[:, base + h, ds_], h == 0, h == nh - 1,
                                     [OT.part((base + h, i)), WB.part(base + h)], [pb])
                            gsl = gt[:, bi * D + dc * 512: bi * D + (dc + 1) * 512]
                            if bi == 0:
                                k.tt("dve", mg[:, ds_], pb[:], gsl, ALU.mult, [pb, gt], [mg])
                            else:
                                k.tt("dve", mgt[:], pb[:], gsl, ALU.mult, [pb, gt], [mgt])
                                k.tt("dve", mg[:, ds_], mg[:, ds_], mgt[:], ALU.add, [mg, mgt], [mg])
                    if debug and "mg" in dbg:
                        k.dmaq("sp", dbg["mg"][rows, :], mg[:], reads=[mg])
                    k.cp("act", mgb[:], mg[:], [mg], [mgb])
                    for half in range(2):
                        pb = PB[pcn % 2]
                        pcn += 1
                        pv = pb[:].bitcast(BF16).rearrange("p (c t) -> p c t", c=8)
                        for c in range(8):
                            kc = half * 8 + c
                            k.tr(pv[:, c, :], mgb[:, kc * 128:(kc + 1) * 128], identB[:], [mgb, identB], [pb])
                        k.cp("dve", MT[:, half * 8:(half + 1) * 8, rows], pv, [pb], [MT.part(i)])
                barrier()
            k.es = es3
            OT_guard.__exit__(None, None, None)
            esR = ExitStack()
            H2 = Buf(esR.enter_context(nc.sbuf_tensor("H2", [128, NQ, D], BF16, side="right")))
            CO = Buf(esR.enter_context(nc.sbuf_tensor("CO", [128, NQ, E], F32, side="right")))
            AM = Buf(esR.enter_context(nc.sbuf_tensor("AM", [128, NQ, E], F32, side="right")))
            POS = Buf(esR.enter_context(nc.sbuf_tensor("POS", [128, NQ, E], F32, side="right")))
            with ExitStack() as es3b:
                k.es = es3b
                WO = k.sb("WO", [128, KC, D], BF16)
                WR = k.sb("WR", [128, KC, NR], F32)
                for kc in range(KC):
                    for hf in range(2):
                        st = stg[sc_ % 2]
                        sc_ += 1
                        k.dmaq("sp", st[:], wout_d[kc * 128:(kc + 1) * 128, hf * 1024:(hf + 1) * 1024], writes=[st])
                        k.cp("dve" if sc_ % 2 else "act", WO[:, kc, hf * 1024:(hf + 1) * 1024], st[:], [st], [WO.part(kc)])
                wrs = k.sb("wrs", [128, KC, NR], F32)
                k.dmaq("sp", wrs[:], wr_d.rearrange("(kc p) c -> p kc c", p=128), writes=[wrs])
                for kc in range(KC):
                    k.ts("dve", WR[:, kc, :], wrs[:, kc, :], gcols[:, gFf + kc:gFf + kc + 1], None, ALU.mult, None,
                         [wrs, gcols], [WR])
                triS = k.sb("triS", [128, 128], F32)
                carryE = k.sb("carryE", [128, E], F32)
                k.op("pool", lambda q: q.memset(triS[:], 1.0), [], [triS])
                k.op("pool", lambda q: q.affine_select(out=triS[:], in_=triS[:], pattern=[[1, 128]], compare_op=ALU.is_gt,
                                                       fill=0.0, base=0, channel_multiplier=-1), [triS], [triS])
                k.op("pool", lambda q: q.memset(carryE[:], 0.0), [], [carryE])
                xo_sb = k.sb("xo_sb", [128, D], F32)
                x1 = k.sb("x1", [128, D], F32)
                h2f = k.sb("h2f", [128, D], F32)
                h2fT = k.sb("h2fT", [128, KC, 128], F32)
                ss1 = k.sb("ss1b", [128, 1], F32)
                lg = k.sb("lg", [128, NR], F32)
                rt = k.sb("rt", [128, 8 * E + 64], F32)
                for i in range(NQ):
                    rows = slice(i * 128, (i + 1) * 128)
                    k.dmaq("sp", xo_sb[:], xo[rows, :], writes=[xo_sb])
                    for dc in range(4):
                        ds_ = slice(dc * 512, (dc + 1) * 512)
                        pb = PB[2 + pcn % 3]
                        pcn += 1
                        for kc in range(KC):
                            k.mm(pb[:], MT[:, kc, rows], WO[:, kc, ds_], kc == 0, kc == KC - 1, [MT.part(i), WO.part(kc)], [pb])
                        k.tt("dve", x1[:, ds_], pb[:], xo_sb[:, ds_], ALU.add, [pb, xo_sb], [x1])
                    k.dmaq("pool", x1sc[rows, :], x1[:], reads=[x1], writes=[t_x1])
                    if debug and "x1" in dbg:
                        k.dmaq("sp", dbg["x1"][rows, :], x1[:], reads=[x1])
                    k.act(h2f[:], x1[:], AF.Square, [x1], [h2f, ss1], accum_out=ss1[:])
                    k.ts("dve", ss1[:], ss1[:], 1.0 / D, EPS, ALU.mult, ALU.add, [ss1], [ss1])
                    k.act(ss1[:], ss1[:], AF.Sqrt, [ss1], [ss1])
                    k.op("dve", lambda q: q.reciprocal(out=ss1[:], in_=ss1[:]), [ss1], [ss1])
                    k.ts("dve", h2f[:], x1[:], ss1[:, 0:1], None, ALU.mult, None, [x1, ss1], [h2f])
                    for q4 in range(4):
                        pb = PB[5 + pcn % 2]
                        pcn += 1
                        for c in range(4):
                            kc = q4 * 4 + c
                            k.tr(pb[:, c * 128:(c + 1) * 128], h2f[:, kc * 128:(kc + 1) * 128], identF[:], [h2f, identF], [pb])
                        k.cp("act", h2fT[:, q4 * 4:(q4 + 1) * 4, :], pb[:].rearrange("p (c t) -> p c t", c=4), [pb], [h2fT])
                    k.cp("pool", H2[:, i, :], h2f[:], [h2f], [H2.part(i)])
                    pb = PB[7]
                    for kc in range(KC):
                        k.mm(pb[:, 0:NR], h2fT[:, kc, :], WR[:, kc, :], kc == 0, kc == KC - 1, [h2fT, WR], [pb])
                    k.tt("dve", lg[:], pb[:, 0:NR], RB, ALU.add, [pb, gh], [lg])
                    gmax = rt[:, 0:1]
                    ngmax = rt[:, 1:2]
                    gsum = rt[:, 2:3]
                    gex = rt[:, 4:4 + G]
                    ohg = rt[:, 16:16 + G]
                    pen = rt[:, 32:32 + G]
                    o0 = 64
                    em = rt[:, o0:o0 + E]
                    oh1 = rt[:, o0 + E:o0 + 2 * E]
                    oh2 = rt[:, o0 + 2 * E:o0 + 3 * E]
                    em2 = rt[:, o0 + 3 * E:o0 + 4 * E]
                    m1 = rt[:, 40:41]
                    m2 = rt[:, 41:42]
                    dd_ = rt[:, 42:43]
                    w1 = rt[:, 43:44]
                    w2 = rt[:, 44:45]
                    R = [rt, lg]
                    k.op("dve", lambda q: q.tensor_reduce(out=gmax, in_=lg[:, 0:G], axis=AX.X, op=ALU.max), [lg], [rt])
                    k.ts("dve", ngmax, gmax, -1.0, None, ALU.mult, None, [rt], [rt])
                    k.act(gex, lg[:, 0:G], AF.Exp, R, [rt], bias=ngmax, accum_out=gsum)
                    k.op("dve", lambda q: q.reciprocal(out=gsum, in_=gsum), [rt], [rt])
                    k.ts("dve", ohg, lg[:, 0:G], gmax, None, ALU.is_equal, None, R, [rt])
                    k.ts("dve", pen, ohg, -1.0, 1e9, ALU.add, ALU.mult, [rt], [rt])
                    k.tt("dve", em.rearrange("p (g e) -> p g e", g=G), lg[:, G:NR].rearrange("p (g e) -> p g e", g=G),
                         pen.unsqueeze(2).to_broadcast([128, G, EPG]), ALU.add, R, [rt])
                    k.op("dve", lambda q: q.tensor_reduce(out=m1, in_=em, axis=AX.X, op=ALU.max), [rt], [rt])
                    k.ts("dve", oh1, em, m1, None, ALU.is_equal, None, [rt], [rt])
                    k.stt(em2, oh1, -1e9, em, ALU.mult, ALU.add, [rt], [rt])
                    k.op("dve", lambda q: q.tensor_reduce(out=m2, in_=em2, axis=AX.X, op=ALU.max), [rt], [rt])
                    k.ts("dve", oh2, em2, m2, None, ALU.is_equal, None, [rt], [rt])
                    k.tt("dve", AM[:, i, :], oh1, oh2, ALU.add, [rt], [AM.part(i)])
                    pcs = PB[6]
                    k.mm(pcs[:, 0:E], triS[:], AM[:, i, :], True, True, [triS, AM.part(i)], [pcs])
                    k.tt("dve", POS[:, i, :], pcs[:, 0:E], carryE[:], ALU.add, [pcs, carryE], [POS.part(i)])
                    k.mm(pcs[:, 64:64 + E], onesF[:], AM[:, i, :], True, True, [onesF, AM.part(i)], [pcs])
                    k.tt("dve", carryE[:], pcs[:, 64:64 + E], carryE[:], ALU.add, [pcs, carryE], [carryE])
                    k.tt("dve", dd_, m2, m1, ALU.subtract, [rt], [rt])
                    k.act(dd_, dd_, AF.Exp, [rt], [rt])
                    k.ts("dve", w1, dd_, 1.0, None, ALU.add, None, [rt], [rt])
                    k.op("dve", lambda q: q.reciprocal(out=w1, in_=w1), [rt], [rt])
                    k.tt("dve", w2, dd_, w1, ALU.mult, [rt], [rt])
                    k.tt("dve", w1, w1, gsum, ALU.mult, [rt], [rt])
                    k.tt("dve", w2, w2, gsum, ALU.mult, [rt], [rt])
                    k.ts("dve", oh1, oh1, w1, None, ALU.mult, None, [rt], [rt])
                    k.stt(CO[:, i, :], oh2, w2, oh1, ALU.mult, ALU.add, [rt], [CO.part(i)])
                    if debug and "co" in dbg:
                        k.dmaq("sp", dbg["co"][rows, :], CO[:, i, :], reads=[CO.part(i)])
                barrier()
            k.es = es3
            barrier()
        k.es = es

        CAP = 128
        EG = 2
        with ExitStack() as es4:
            k.es = es4
            Y = k.sb("Y", [128, NQ, D], F32)
            iotaC = k.sb("iotaC", [128, CAP], F32)
            k.op("pool", lambda q: q.iota(iotaC[:], pattern=[[1, CAP]], base=0, channel_multiplier=0,
                                          allow_small_or_imprecise_dtypes=True), [], [iotaC])
            st4 = [k.sb(f"st4_{i}", [128, 2, 1024], F32) for i in range(4)]
            wb4 = [k.sb(f"wb4_{i}", [128, 2, 1024], BF16) for i in range(6)]
            SG = k.sb("SG", [128, NQ, CAP], BF16)
            SW = k.sb("SW", [128, NQ, CAP], BF16)
            XS = k.sb("XS", [128, KC, CAP], BF16)
            XSs = k.sb("XSs", [128, D], BF16)
            sg = [k.sb(f"sg{i}", [128, 512], F32) for i in range(2)]
            HD = k.sb("HD", [128, FF], BF16)
            HT = k.sb("HT", [128, FF // 128, CAP], BF16)
            YE = [k.sb(f"YE{g}", [128, D], BF16) for g in range(EG)]
            ST = [k.sb(f"ST{g}", [128, T], BF16) for g in range(EG)]
            c4 = {"s": 0, "x": 0}
            firstg = True

            def stream(src_rows_ap, kind, gofs):
                st = st4[c4["s"] % 4]
                wb = wb4[c4["s"] % 6]
                n0 = c4["s"]
                c4["s"] += 1
                k.dmaq("sp", st[:], src_rows_ap, writes=[st])
                for c in range(2):
                    for hf in range(2):
                        eng = ("act", "dve")[(2 * c + hf + n0) % 2]
                        hs = slice(hf * 512, (hf + 1) * 512)
                        if kind == "gu":
                            scast(eng, wb[:, c, hs], st[:, c, hs], gcols[:, gofs + c:gofs + c + 1], [st, gcols], [wb.part(2 * c + hf)])
                        else:
                            k.cp(eng, wb[:, c, hs], st[:, c, hs], [st], [wb.part(2 * c + hf)])
                return wb

            SW2 = [SW, k.sb("SWb", [128, NQ, CAP], BF16)]

            def prep_A(e):
                sw = SW2[e % 2]
                for i in range(NQ):
                    k.ts("dve", SG[:, i, :], iotaC[:], POS[:, i, e:e + 1], AM[:, i, e:e + 1], ALU.is_equal, ALU.mult,
                         [iotaC, POS.part(i), AM.part(i)], [SG.part(i)])
                    k.ts("pool", sw[:, i, :], iotaC[:], POS[:, i, e:e + 1], CO[:, i, e:e + 1], ALU.is_equal, ALU.mult,
                         [iotaC, POS.part(i), CO.part(i)], [sw.part(i)])

            def gather_dc(dc):
                pb = PB[4 + dc % 2]
                for i in range(NQ):
                    k.mm(pb[:], SG[:, i, :], H2[:, i, dc * 512:(dc + 1) * 512], i == 0, i == NQ - 1,
                         [H2.part(i), SG.part(i)], [pb])
                k.cp("act" if dc % 2 else "dve", XSs[:, dc * 512:(dc + 1) * 512], pb[:], [pb], [XSs])

            def gather_tr(half):
                pb = PB[6 + half]
                pv = pb[:].bitcast(BF16).rearrange("p (c t) -> p c t", c=8)
                for c in range(8):
                    kc = half * 8 + c
                    k.tr(pv[:, c, :], XSs[:, kc * 128:(kc + 1) * 128], identB[:], [XSs, identB], [pb])
                k.cp("dve" if half else "act", XS[:, half * 8:(half + 1) * 8, :], pv, [pb], [XS.part(half * 2), XS.part(half * 2 + 1)])

            sc_state = {"first": True}

            def scatter_tile(i, dc):
                ds_ = slice(dc * 512, (dc + 1) * 512)
                pb = PB[4 + c4["x"] % 2]
                c4["x"] += 1
                for gg in range(EG):
                    k.mm(pb[:], ST[gg][:, i * 128:(i + 1) * 128], YE[gg][:, ds_], gg == 0, gg == EG - 1,
                         [ST[gg], YE[gg]], [pb])
                if sc_state["first"]:
                    k.cp("dve", Y[:, i, ds_], pb[:], [pb], [Y.part((i, dc))])
                else:
                    k.tt("dve", Y[:, i, ds_], pb[:], Y[:, i, ds_], ALU.add, [pb, Y.part((i, dc))], [Y.part((i, dc))])

            prep_A(0)
            for dc in range(4):
                gather_dc(dc)
            for half in range(2):
                gather_tr(half)
            for e in range(E):
                g = e % EG
                pend_sc = []
                if g == 0 and e > 0:
                    pend_sc = [(i, dc) for i in range(NQ) for dc in range(4)]
                nsl = 2 * (KC // 2)
                per = -(-len(pend_sc) // nsl) if pend_sc else 0
                for (src, base) in ((wg_d, 0), (wu_d, 2)):
                    sv = src[e].rearrange("(kc p) f -> p kc f", p=128)
                    for k2 in range(KC // 2):
                        wb = stream(sv[:, k2 * 2:(k2 + 1) * 2, :], "gu", gFf + k2 * 2)
                        for c in range(2):
                            kc = k2 * 2 + c
                            for fh in range(2):
                                k.mm(PB[base + fh][:], XS[:, kc, :], wb[:, c, fh * 512:(fh + 1) * 512], kc == 0, kc == KC - 1,
                                     [XS.part(kc // 4), wb.part(2 * c + fh)], [PB[base + fh]])
                        for _ in range(per):
                            if pend_sc:
                                scatter_tile(*pend_sc.pop(0))
                while pend_sc:
                    scatter_tile(*pend_sc.pop(0))
                if g == 0 and e > 0:
                    sc_state["first"] = False
                for fh in range(2):
                    sgb = sg[fh]
                    k.act(sgb[:], PB[fh][:], AF.Silu, [PB[fh]], [sgb])
                    k.tt("dve", HD[:, fh * 512:(fh + 1) * 512], sgb[:], PB[2 + fh][:], ALU.mult, [sgb, PB[2 + fh]], [HD])
                pb = PB[6]
                pv = pb[:].bitcast(BF16).rearrange("p (c t) -> p c t", c=8)
                for c in range(FF // 128):
                    k.tr(pv[:, c, :], HD[:, c * 128:(c + 1) * 128], identB[:], [HD, identB], [pb])
                k.cp("act", HT[:], pv, [pb], [HT])
                dv_ = wd_d[e].rearrange("(fc p) d -> p fc d", p=128)
                for fc in range(FF // 128):
                    st = st4[c4["s"] % 4]
                    wb = wb4[c4["s"] % 6]
                    n0 = c4["s"]
                    c4["s"] += 1
                    sflat = st[:].rearrange("p a b -> p (a b)")
                    wflat = wb[:].rearrange("p a b -> p (a b)")
                    k.dmaq("sp", sflat, dv_[:, fc, :], writes=[st])
                    for c in range(4):
                        eng = ("act", "dve")[(c + n0) % 2]
                        k.cp(eng, wflat[:, c * 512:(c + 1) * 512], sflat[:, c * 512:(c + 1) * 512], [st], [wb.part(c)])
                    for dc in range(4):
                        k.mm(PB[dc][:], HT[:, fc, :], wflat[:, dc * 512:(dc + 1) * 512], fc == 0, fc == FF // 128 - 1,
                             [HT, wb.part(dc)], [PB[dc]])
                    if e + 1 < E:
                        if fc == 0:
                            prep_A(e + 1)
                        elif 1 <= fc <= 4:
                            gather_dc(fc - 1)
                        elif fc in (5, 6):
                            gather_tr(fc - 5)
                for dc in range(4):
                    k.cp("act" if dc % 2 else "dve", YE[g][:, dc * 512:(dc + 1) * 512], PB[dc][:], [PB[dc]], [YE[g]])
                sw = SW2[e % 2]
                pb = PB[7]
                pv = pb[:].bitcast(BF16).rearrange("p (c t) -> p c t", c=8)
                for i0 in range(0, NQ, 8):
                    n_ = min(8, NQ - i0)
                    for c in range(n_):
                        k.tr(pv[:, c, :], sw[:, i0 + c, :], identB[:], [sw.part(i0 + c), identB], [pb])
                    k.cp("dve", ST[g][:, i0 * 128:(i0 + n_) * 128].rearrange("p (c t) -> p c t", c=n_), pv[:, 0:n_, :], [pb], [ST[g]])
            for i in range(NQ):
                for dc in range(4):
                    scatter_tile(i, dc)
            for tb in range(NQ):
                rows = slice(tb * 128, (tb + 1) * 128)
                xb_ = st4[tb % 4]
                xv = xb_[:].rearrange("p a b -> p (a b)")
                k.dmaq("sp", xv, x1sc[rows, :], reads=[t_x1], writes=[xb_])
                k.tt("dve", xv, xv, Y[:, tb, :], ALU.add, [xb_] + [Y.part((tb, dc)) for dc in range(4)], [xb_])
                k.dmaq("sp", out_d[rows, :], xv, reads=[xb_])
            e_sp = k.engs["sp"]
            for qn in k.dma:
                for key, val in k.dma[qn][0]:
                    if val > 0:
                        e_sp.q.wait_ge(k.sems[key], val)
            barrier()
        k.es = es
        esR.close()
        print("instructions:", k.nins, "sems:", k.nsem)
    return nc


def host_inputs(cfg, core, x, mem, positions, attn_norm_g, mem_norm_g, w_in, fox_forget_b,
                fox_q_norm_g, fox_k_norm_g, dsa_q_norm_g, dsa_k_norm_g, idx_k_norm_g,
                cross_q_norm_g, cross_k_norm_g, w_mem_kv, w_branch_fox, w_branch_dsa,
                w_branch_cross, w_out, ffn_norm_g, router_group_w, router_group_b,
                router_expert_w, router_expert_b, expert_w_gate, expert_w_up, expert_w_down, shared):
    S, NQ = cfg.S, cfg.NQ
    b, j = core // 4, core % 4
    f32 = np.float32
    own = np.concatenate([np.arange((4 * i + j) * 128, (4 * i + j + 1) * 128) for i in range(NQ)])
    if "wk" not in shared:
        W = np.asarray(w_in[0], f32)
        o = np.cumsum([0, 768, 768, 768, 6, 768, 768, 768, 1024, 16, 64, 512, 3 * D])
        seg = {n: W[:, o[i]:o[i + 1]] for i, n in enumerate(["fq", "fk", "fv", "fl", "dq", "dk", "dv", "iq", "iw", "ik", "cq", "gt"])}
        shared["wk"] = np.ascontiguousarray(np.concatenate([seg["fk"], seg["dk"], seg["fv"], seg["dv"], seg["ik"], seg["fl"]], axis=1))
        shared["wq"] = np.ascontiguousarray(np.concatenate([seg["fq"], seg["dq"], seg["cq"], seg["iq"], seg["iw"], seg["gt"]], axis=1))
        shared["wr"] = np.ascontiguousarray(np.concatenate([np.asarray(router_group_w[0], f32), np.asarray(router_expert_w[0], f32)], axis=1))
        gc = [np.asarray(g[0], f32).reshape(KC, 128).T for g in (attn_norm_g, mem_norm_g, ffn_norm_g)]
        shared["gcols"] = np.ascontiguousarray(np.concatenate(gc, axis=1))
        half16 = np.power(np.float32(500000.0), -np.arange(16, dtype=f32) * 2.0 / 32).astype(f32)
        half8 = np.power(np.float32(500000.0), -np.arange(8, dtype=f32) * 2.0 / 16).astype(f32)
        ifr = (np.concatenate([half16, half8]).astype(np.float64) / (2 * np.pi)).astype(f32)
        shared["gh"] = np.concatenate([np.asarray(v[0], f32).reshape(-1) for v in
                                       (fox_q_norm_g, fox_k_norm_g, dsa_q_norm_g, dsa_k_norm_g, cross_q_norm_g,
                                        cross_k_norm_g, idx_k_norm_g, fox_forget_b, router_group_b, router_expert_b)] + [ifr]).astype(f32)
        shared["ident"] = np.eye(128, dtype=f32)
        for nm, arr in (("wmem", w_mem_kv), ("wbf", w_branch_fox), ("wbd", w_branch_dsa), ("wbc", w_branch_cross),
                        ("wout", w_out), ("wg", expert_w_gate), ("wu", expert_w_up), ("wd", expert_w_down)):
            shared[nm] = np.ascontiguousarray(np.asarray(arr[0], f32))
    kk = np.arange(128)[:, None]
    qq = np.arange(128)[None, :]
    cm = np.zeros((128, 4, 128), f32)
    cmT = np.zeros((128, 4, 128), f32)
    sel = np.zeros((128, 4, 128), f32)
    for r in range(4):
        if r == j:
            cm[:, r, :] = np.where(kk <= qq, 0.0, NEG)
            cmT[:, r, :] = np.where(qq >= kk, 0.0, NEG).T if False else np.where(kk >= qq, 0.0, NEG)
            sel[64, r, :] = 1.0
        elif r > j:
            cm[:, r, :] = NEG
            cmT[:, r, :] = NEG
    m = dict(
        xb=np.ascontiguousarray(np.asarray(x[b], f32)),
        xo=np.ascontiguousarray(np.asarray(x[b], f32)[own]),
        memb=np.ascontiguousarray(np.asarray(mem[b], f32)),
        posb2=np.ascontiguousarray(np.asarray(positions[b], np.int32).reshape(cfg.NB, 128).T),
        poso2=np.ascontiguousarray(np.asarray(positions[b], np.int32)[own].reshape(NQ, 128).T),
        cm=cm, cmT=cmT, sel=sel,
    )
    for nm in ("wk", "wq", "wr", "gcols", "gh", "ident", "wmem", "wbf", "wbd", "wbc", "wout", "wg", "wu", "wd"):
        m[nm] = shared[nm]
    return m, own


_NC_CACHE = {}


def run(cfg, inputs, debug=False, ncores=8):
    key = (cfg.S, cfg.G, cfg.EPG, bool(debug))
    if key not in _NC_CACHE:
        _NC_CACHE[key] = build_nc(cfg, debug)
    nc = _NC_CACHE[key]
    shared = {}
    maps, owns = [], []
    for c in range(ncores):
        m, own = host_inputs(cfg, c, shared=shared, **inputs)
        maps.append(m)
        owns.append(own)
    res = run_bass_kernel_spmd(nc, maps, core_ids=list(range(ncores)))
    B = inputs["x"].shape[0]
    out = np.zeros((B, cfg.S, D), np.float32)
    for c in range(ncores):
        out[c // 4, owns[c], :] = res.results[c]["out"]
    return out, res


def kernel(**inputs):
    cfg = Cfg()
    out, _ = run(cfg, inputs)
    return out
```

```python
import numpy as np
from contextlib import ExitStack
import concourse.bass as bass
import concourse.mybir as mybir
from concourse.bass_utils import run_bass_kernel_spmd

F32 = mybir.dt.float32
BF16 = mybir.dt.bfloat16
I32 = mybir.dt.int32
U32 = mybir.dt.uint32
AF = mybir.ActivationFunctionType
ALU = mybir.AluOpType
AX = mybir.AxisListType


class Trk:
    __slots__ = ("w", "r")

    def __init__(self):
        self.w = None
        self.r = {}


class Buf:
    def __init__(self, h):
        self.h = h
        self.t = Trk()
        self.parts = {}

    def __getitem__(self, key):
        return self.h[key]

    def part(self, i):
        p = self.parts.get(i)
        if p is None:
            p = self.parts[i] = Trk()
        return p


def _trk(x):
    return x.t if isinstance(x, Buf) else x


class Eng:
    def __init__(self, name, q, selfsync):
        self.name = name
        self.q = q
        self.selfsync = selfsync
        self.sem = None
        self.semkey = None
        self.cnt = 0
        self.seen = {}


class KB:
    SEM_EPOCH = 20000
    NDMA = 8

    def __init__(self, nc, es):
        self.nc = nc
        self.es = es
        self.sems = {}
        self.nsem = 0
        self.engs = {
            "pe": Eng("pe", nc.tensor, False),
            "act": Eng("act", nc.scalar, True),
            "dve": Eng("dve", nc.vector, True),
            "pool": Eng("pool", nc.gpsimd, True),
            "sp": Eng("sp", nc.sync, False),
        }
        for e in self.engs.values():
            self._new_sem(e)
        self.dma = {}
        for qn in ("sp", "pool", "act"):
            ring = []
            for i in range(self.NDMA):
                key = self._alloc_sem(f"d{qn}{i}")
                ring.append([key, 0])
            self.dma[qn] = [ring, 0]
        self.nins = 0

    def _alloc_sem(self, name):
        s = self.es.enter_context(self.nc.semaphore(f"{name}_{self.nsem}"))
        key = self.nsem
        self.nsem += 1
        self.sems[key] = s
        return key

    def _new_sem(self, e):
        e.semkey = self._alloc_sem(e.name)
        e.sem = self.sems[e.semkey]
        e.cnt = 0

    def sb(self, name, shape, dtype):
        h = self.es.enter_context(self.nc.sbuf_tensor("s_" + name, list(shape), dtype))
        return Buf(h)

    def ps(self, name, shape, dtype=F32):
        h = self.es.enter_context(self.nc.psum_tensor("p_" + name, list(shape), dtype))
        return Buf(h)

    def _waits(self, e, reads, writes):
        need = {}

        def add(ev, same_ok):
            if ev is None:
                return
            k, v = ev
            if k == e.semkey and not same_ok:
                return
            if e.seen.get(k, 0) >= v:
                return
            if need.get(k, 0) < v:
                need[k] = v

        for t in reads:
            t = _trk(t)
            add(t.w, e.selfsync)
        for t in writes:
            t = _trk(t)
            add(t.w, e.selfsync)
            for k, v in t.r.items():
                add((k, v), False)
        for k, v in need.items():
            e.q.wait_ge(self.sems[k], v)
            e.seen[k] = v

    def _mark(self, ev, reads, writes):
        k, v = ev
        for t in reads:
            t = _trk(t)
            if t.r.get(k, 0) < v:
                t.r[k] = v
        for t in writes:
            t = _trk(t)
            t.w = ev
            t.r = {}

    def op(self, eng, fn, reads=(), writes=()):
        e = self.engs[eng]
        self._waits(e, reads, writes)
        ins = fn(e.q)
        e.cnt += 1
        ins.then_inc(e.sem, 1)
        ev = (e.semkey, e.cnt)
        self._mark(ev, reads, writes)
        if e.cnt >= self.SEM_EPOCH:
            self._new_sem(e)
        self.nins += 1
        return ins

    def dmaq(self, qn, out, in_, reads=(), writes=(), **kw):
        e = self.engs[qn]
        self._waits(e, reads, writes)
        ring, pos = self.dma[qn]
        slot = ring[pos % self.NDMA]
        self.dma[qn][1] = pos + 1
        key, val = slot
        if val > 0 and e.seen.get(key, 0) < val:
            e.q.wait_ge(self.sems[key], val)
            e.seen[key] = val
        ins = e.q.dma_start(out=out, in_=in_, **kw)
        val += 16
        slot[1] = val
        ins.then_inc(self.sems[key], 16)
        ev = (key, val)
        self._mark(ev, reads, writes)
        self.nins += 1
        return ev

    def wait_all(self, eng, trks):
        e = self.engs[eng]
        self._waits(e, trks, trks)

    def mm(self, out, lhsT, rhs, start, stop, reads, writes, **kw):
        return self.op("pe", lambda q: q.matmul(out, lhsT, rhs, start=start, stop=stop, **kw), reads, writes)

    def tr(self, out, in_, ident, reads, writes):
        return self.op("pe", lambda q: q.transpose(out, in_, ident), reads, writes)

    def act(self, out, in_, func, reads, writes, eng="act", **kw):
        return self.op(eng, lambda q: q.activation(out=out, in_=in_, func=func, **kw), reads, writes)

    def tt(self, eng, out, in0, in1, op, reads, writes):
        return self.op(eng, lambda q: q.tensor_tensor(out=out, in0=in0, in1=in1, op=op), reads, writes)

    def ts(self, eng, out, in0, s1, s2, op0, op1, reads, writes, **kw):
        if op1 is None:
            return self.op(eng, lambda q: q.tensor_scalar(out=out, in0=in0, scalar1=s1, scalar2=None, op0=op0, **kw), reads, writes)
        return self.op(eng, lambda q: q.tensor_scalar(out=out, in0=in0, scalar1=s1, scalar2=s2, op0=op0, op1=op1, **kw), reads, writes)

    def stt(self, out, in0, scalar, in1, op0, op1, reads, writes):
        return self.op("dve", lambda q: q.scalar_tensor_tensor(out=out, in0=in0, scalar=scalar, in1=in1, op0=op0, op1=op1), reads, writes)

    def cp(self, eng, out, in_, reads, writes):
        if eng == "act":
            return self.op(eng, lambda q: q.copy(out=out, in_=in_), reads, writes)
        return self.op(eng, lambda q: q.tensor_copy(out=out, in_=in_), reads, writes)


D = 2048
KC = D // 128
DH = 128
FH = 6
CH = 4
NMEM = 256
IH = 16
IDD = 64
FF = 1024
EPS = 1e-6
TOPK = 256
NEG = -30000.0
TWO_PI = 2.0 * np.pi

K_FK, K_DK, K_FV, K_DV, K_IK, K_FL = 0, 768, 1536, 2304, 3072, 3136
CK = 3142
Q_FQ, Q_DQ, Q_CQ, Q_IQ, Q_IW, Q_GT = 0, 768, 1536, 2048, 3072, 3088
CQ = 3088 + 3 * D


class Cfg:
    def __init__(self, S=4096, G=4, EPG=8):
        self.S = S
        self.NB = S // 128
        self.NQ = self.NB // 4
        self.T = self.NQ * 128
        self.G = G
        self.EPG = EPG
        self.E = G * EPG
        self.topk = min(TOPK, S // 4)


def build_nc(cfg, debug=False):
    S, NB, NQ, T, E, G, EPG = cfg.S, cfg.NB, cfg.NQ, cfg.T, cfg.E, cfg.G, cfg.EPG
    NR = G + E
    nc = bass.Bass("TRN2", target_bir_lowering=False)

    def din(name, shape, dt=F32):
        return nc.dram_tensor(name, list(shape), dt, kind="ExternalInput").ap()

    xb = din("xb", [S, D])
    xo = din("xo", [T, D])
    memb = din("memb", [NMEM, D])
    posb2 = din("posb2", [128, NB], I32)
    poso2 = din("poso2", [128, NQ], I32)
    wk = din("wk", [D, CK])
    wq = din("wq", [D, CQ])
    wmem = din("wmem", [D, 2 * CH * DH])
    wbf_d = din("wbf", [FH * DH, D])
    wbd_d = din("wbd", [FH * DH, D])
    wbc_d = din("wbc", [CH * DH, D])
    wout_d = din("wout", [D, D])
    wr_d = din("wr", [D, NR])
    wg_d = din("wg", [E, D, FF])
    wu_d = din("wu", [E, D, FF])
    wd_d = din("wd", [E, FF, D])
    gcols_d = din("gcols", [128, 3 * KC])
    gh_d = din("gh", [6 * 128 + 64 + 6 + NR + 24])
    ident_d = din("ident", [128, 128])
    cm_d = din("cm", [128, 4, 128])
    cmT_d = din("cmT", [128, 4, 128])
    sel_d = din("sel", [128, 4, 128])
    out_d = nc.dram_tensor("out", [T, D], F32, kind="ExternalOutput").ap()
    dbg = {}
    if debug:
        for nm, shp in debug.items():
            dbg[nm] = nc.dram_tensor("dbg_" + nm, list(shp), F32, kind="ExternalOutput").ap()

    ksc = nc.dram_tensor("ksc", [S, CK], F32).ap()
    qsc = nc.dram_tensor("qsc", [T, CQ], F32).ap()
    msc = nc.dram_tensor("msc", [NMEM, 2 * CH * DH], F32).ap()
    x1sc = nc.dram_tensor("x1sc", [T, D], F32).ap()
    t_ksc, t_qsc, t_msc, t_x1 = Trk(), Trk(), Trk(), Trk()

    with ExitStack() as es:
        k = KB(nc, es)
        PB = [k.ps(f"pb{i}", [128, 512], F32) for i in range(8)]

        def pbf(i):
            return PB[i][:].bitcast(BF16)

        identF = k.sb("identF", [128, 128], F32)
        identB = k.sb("identB", [128, 128], BF16)
        onesB = k.sb("onesB", [128, 128], BF16)
        onesF = k.sb("onesF", [128, 128], F32)
        tri = k.sb("tri", [128, 128], F32)
        cmF = k.sb("cmF", [128, 4, 128], F32)
        cmB = k.sb("cmB", [128, 4, 128], BF16)
        cmT = k.sb("cmTs", [128, 4, 128], F32)
        selF = k.sb("selF", [128, 4, 128], F32)
        gcols = k.sb("gcols", [128, 3 * KC], F32)
        NGH = 6 * 128 + 64 + 6 + NR + 24
        gh = k.sb("gh", [128, NGH], F32)
        k.dmaq("sp", identF[:], ident_d[:, :], writes=[identF])
        k.dmaq("sp", cmF[:], cm_d[:, :, :], writes=[cmF])
        k.dmaq("sp", cmT[:], cmT_d[:, :, :], writes=[cmT])
        k.dmaq("sp", selF[:], sel_d[:, :, :], writes=[selF])
        k.dmaq("sp", gcols[:], gcols_d[:, :], writes=[gcols])
        k.dmaq("sp", gh[:], gh_d.partition_broadcast(128), writes=[gh])
        k.cp("dve", identB[:], identF[:], [identF], [identB])
        k.cp("dve", cmB[:], cmF[:], [cmF], [cmB])
        k.op("pool", lambda q: q.memset(onesB[:], 1.0), [], [onesB])
        k.op("pool", lambda q: q.memset(onesF[:], 1.0), [], [onesF])
        k.op("pool", lambda q: q.memset(tri[:], 1.0), [], [tri])
        k.op("pool", lambda q: q.affine_select(out=tri[:], in_=tri[:], pattern=[[1, 128]], compare_op=ALU.is_ge,
                                               fill=0.0, base=0, channel_multiplier=-1), [tri], [tri])
        G_FQ, G_FK, G_DQ, G_DK, G_CQ, G_CK = [gh[:, i * 128:(i + 1) * 128] for i in range(6)]
        G_IK = gh[:, 768:832]
        FB = gh[:, 832:838]
        RB = gh[:, 838:838 + NR]
        IFR = gh[:, 838 + NR:838 + NR + 24]
        gA, gM, gFf = 0, KC, 2 * KC

        small = {}

        def barrier():
            evs = []
            for e in k.engs.values():
                if e.cnt > 0:
                    evs.append((e.semkey, e.cnt))
            for qn in k.dma:
                for key, val in k.dma[qn][0]:
                    if val > 0:
                        evs.append((key, val))
            for e in k.engs.values():
                for key, val in evs:
                    if key == e.semkey:
                        continue
                    if e.seen.get(key, 0) < val:
                        e.q.wait_ge(k.sems[key], val)
                        e.seen[key] = val

        def scast(eng, out, in_, sc_ap, reads, writes):
            if eng == "act":
                k.act(out, in_, AF.Identity, reads, writes, scale=sc_ap)
            elif eng == "pool":
                k.ts("pool", out, in_, sc_ap, 1.0, ALU.mult, ALU.mult, reads, writes)
            else:
                k.ts("dve", out, in_, sc_ap, None, ALU.mult, None, reads, writes)

        def rstd_from_ss(ss, n, inv_n):
            k.ts("dve", ss, ss, inv_n, EPS, ALU.mult, ALU.add, [small["ss"]], [small["ss"]])
            k.act(ss, ss, AF.Sqrt, [small["ss"]], [small["ss"]])
            k.op("dve", lambda q: q.reciprocal(out=ss, in_=ss), [small["ss"]], [small["ss"]])

        def head_norm(srcbuf, src, H, dd, gain, dstbuf, dst, sqbuf):
            ssb = small["ss"]
            ss = ssb[:, 0:H]
            k.tt("dve", sqbuf[:, 0:H * dd], src, src, ALU.mult, [srcbuf], [sqbuf])
            k.op("dve", lambda q: q.tensor_reduce(out=ss, in_=sqbuf[:, 0:H * dd].rearrange("p (h d) -> p h d", h=H),
                                                  axis=AX.X, op=ALU.add), [sqbuf], [ssb])
            rstd_from_ss(ss, H, 1.0 / dd)
            s3 = src.rearrange("p (h d) -> p h d", h=H)
            d3 = dst.rearrange("p (h d) -> p h d", h=H)
            k.tt("dve", d3, s3, ss.unsqueeze(2).to_broadcast([128, H, dd]), ALU.mult, [srcbuf, ssb], [dstbuf])
            k.tt("dve", d3, d3, gain.unsqueeze(1).to_broadcast([128, H, dd]), ALU.mult, [dstbuf], [dstbuf])

        def sincos(pos_ap_dram, csbuf):
            pi_ = small["posi"]
            pf = small["posf"]
            a = small["ang"]
            ki = small["angi"]
            kf = small["angf"]
            m = small["angm"]
            k.dmaq("sp", pi_[:], pos_ap_dram, writes=[pi_])
            k.cp("dve", pf[:], pi_[:], [pi_], [pf])
            k.ts("dve", a[:], IFR, pf[:, 0:1], None, ALU.mult, None, [pf, gh], [a])
            k.cp("dve", ki[:], a[:], [a], [ki])
            k.cp("dve", kf[:], ki[:], [ki], [kf])
            k.tt("dve", a[:], a[:], kf[:], ALU.subtract, [a, kf], [a])

            def wrap(dst, src, shift, rd):
                if shift != 0.0:
                    k.ts("dve", dst, src, shift, None, ALU.add, None, rd, [csbuf])
                    src = dst
                    rd = [csbuf]
                k.ts("dve", m[:], src, 0.5, None, ALU.is_ge, None, rd, [m])
                k.tt("dve", dst, src, m[:], ALU.subtract, rd + [m], [csbuf])
                k.ts("dve", m[:], dst, -0.5, None, ALU.is_lt, None, [csbuf], [m])
                k.tt("dve", dst, dst, m[:], ALU.add, [csbuf, m], [csbuf])
            sn = csbuf[:, 24:48]
            cs = csbuf[:, 0:24]
            wrap(sn, a[:], 0.0, [a])
            wrap(cs, sn, 0.25, [csbuf])
            k.act(csbuf[:, 0:48], csbuf[:, 0:48], AF.Sin, [csbuf], [csbuf], scale=TWO_PI)

        def rope(buf, view3, H, half, cos, sin, tmpbuf):
            x1 = view3[:, :, 0:half]
            x2 = view3[:, :, half:2 * half]
            cb = cos.unsqueeze(1).to_broadcast([128, H, half])
            sb_ = sin.unsqueeze(1).to_broadcast([128, H, half])
            t = tmpbuf[:, 0:4 * H * half].rearrange("p (a h d) -> p a h d", a=4, h=H)
            k.tt("dve", t[:, 0], x1, cb, ALU.mult, [buf, small["cs"]], [tmpbuf])
            k.tt("dve", t[:, 1], x2, sb_, ALU.mult, [buf, small["cs"]], [tmpbuf])
            k.tt("dve", t[:, 2], x2, cb, ALU.mult, [buf, small["cs"]], [tmpbuf])
            k.tt("dve", t[:, 3], x1, sb_, ALU.mult, [buf, small["cs"]], [tmpbuf])
            k.tt("dve", x1, t[:, 0], t[:, 1], ALU.subtract, [tmpbuf], [buf])
            k.tt("dve", x2, t[:, 2], t[:, 3], ALU.add, [tmpbuf], [buf])

        with ExitStack() as es1:
            k.es = es1
            small["ss"] = k.sb("ss1", [128, 16], F32)
            TG = min(1024, S)
            hT = k.sb("hT", [128, KC, TG], BF16)
            xin = [k.sb(f"xin{i}", [128, D], F32) for i in range(2)]
            hb = [k.sb(f"hb{i}", [128, D], BF16) for i in range(2)]
            wst = [k.sb(f"wst{i}", [128, KC, 512], F32) for i in range(1)]
            wbfb = [k.sb(f"wbfb{i}", [128, KC, 512], BF16) for i in range(2)]
            osb = [k.sb(f"osb{i}", [128, 512], F32) for i in range(3)]
            ssx = [k.sb(f"ssx{i}", [128, 1], F32) for i in range(2)]
            cnt = {"x": 0, "w": 0, "o": 0, "p": 0}

            def make_hT(src_dram, nblk):
                for tb in range(nblk):
                    i = cnt["x"] % 2
                    cnt["x"] += 1
                    xi, hbi, ssi = xin[i], hb[i], ssx[i]
                    k.dmaq("sp", xi[:], src_dram[tb * 128:(tb + 1) * 128, :], writes=[xi])
                    k.act(hbi[:], xi[:], AF.Square, [xi], [hbi, ssi], accum_out=ssi[:])
                    k.ts("dve", ssi[:], ssi[:], 1.0 / D, EPS, ALU.mult, ALU.add, [ssi], [ssi])
                    k.act(ssi[:], ssi[:], AF.Sqrt, [ssi], [ssi])
                    k.op("dve", lambda q: q.reciprocal(out=ssi[:], in_=ssi[:]), [ssi], [ssi])
                    k.ts("dve", hbi[:], xi[:], ssi[:, 0:1], None, ALU.mult, None, [xi, ssi], [hbi])
                    for half in range(2):
                        pb = PB[cnt["p"] % 2]
                        cnt["p"] += 1
                        pv = pb[:].bitcast(BF16).rearrange("p (c t) -> p c t", c=8)
                        for c in range(8):
                            kc = half * 8 + c
                            k.tr(pv[:, c, :], hbi[:, kc * 128:(kc + 1) * 128], identB[:], [hbi, identB], [pb])
                        eng = "act" if half == 0 else "dve"
                        k.cp(eng, hT[:, half * 8:(half + 1) * 8, tb * 128:(tb + 1) * 128], pv, [pb], [hT.part(tb)])

            def proj(nblk, W, C, gofs, dst, dst_trk):
                chunks = list(range(0, C, 512))
                Wv = W.rearrange("(kc p) c -> p kc c", p=128)
                ws = wst[0]

                def issue_load(c0):
                    cw = min(512, C - c0)
                    k.dmaq("sp", ws[:, :, 0:cw], Wv[:, :, c0:c0 + cw], writes=[ws])
                issue_load(chunks[0])
                for ci, c0 in enumerate(chunks):
                    cw = min(512, C - c0)
                    wb = wbfb[cnt["w"] % 2]
                    cnt["w"] += 1
                    for kc in range(KC):
                        eng = ("dve", "act")[kc % 2]
                        scast(eng, wb[:, kc, 0:cw], ws[:, kc, 0:cw], gcols[:, gofs + kc:gofs + kc + 1], [ws, gcols], [wb.part(kc)])
                    if ci + 1 < len(chunks):
                        issue_load(chunks[ci + 1])
                    for tb in range(nblk):
                        pb = PB[2 + cnt["o"] % 3]
                        ob = osb[cnt["o"] % 3]
                        cnt["o"] += 1
                        for kc in range(KC):
                            k.mm(pb[:, 0:cw], hT[:, kc, tb * 128:(tb + 1) * 128], wb[:, kc, 0:cw], kc == 0, kc == KC - 1,
                                 [hT.part(tb), wb.part(kc)], [pb])
                        k.cp("act", ob[:, 0:cw], pb[:, 0:cw], [pb], [ob])
                        k.dmaq("sp", dst[tb * 128:(tb + 1) * 128, c0:c0 + cw], ob[:, 0:cw], reads=[ob], writes=[dst_trk])

            for grp in range(S // TG):
                make_hT(xb[grp * TG:(grp + 1) * TG, :], TG // 128)
                proj(TG // 128, wk, CK, gA, ksc[grp * TG:(grp + 1) * TG, :], t_ksc)
            make_hT(xo, NQ)
            proj(NQ, wq, CQ, gA, qsc, t_qsc)
            make_hT(memb, NMEM // 128)
            proj(NMEM // 128, wmem, 2 * CH * DH, gM, msc, t_msc)
            barrier()
        k.es = es
        OT_guard = nc.sbuf_tensor("OT", [128, FH + FH + CH, T], BF16, side="right")
        OT = Buf(OT_guard.__enter__())
        mbsc = nc.dram_tensor("mbsc", [NQ, 128, NB * 128], BF16).ap()
        t_mbsc = Trk()

        with ExitStack() as es2:
            k.es = es2
            small["ss"] = k.sb("ss2", [128, 16], F32)
            CSA = k.sb("CSA", [128, NB, 48], F32)
            CSO = k.sb("CSO", [128, NQ, 48], F32)
            with ExitStack() as est:
                k.es = est
                for (pos2, nblk, dst) in ((posb2, NB, CSA), (poso2, NQ, CSO)):
                    pi_ = k.sb(f"pi{nblk}", [128, nblk], I32)
                    pf = k.sb(f"pf{nblk}", [128, nblk], F32)
                    a = k.sb(f"an{nblk}", [128, nblk, 24], F32)
                    ki = k.sb(f"ki{nblk}", [128, nblk, 24], I32)
                    kf = k.sb(f"kf{nblk}", [128, nblk, 24], F32)
                    m = k.sb(f"mm{nblk}", [128, nblk, 24], F32)
                    k.dmaq("sp", pi_[:], pos2[:, :], writes=[pi_])
                    k.cp("dve", pf[:], pi_[:], [pi_], [pf])
                    k.tt("dve", a[:], pf[:].unsqueeze(2).to_broadcast([128, nblk, 24]),
                         IFR.unsqueeze(1).to_broadcast([128, nblk, 24]), ALU.mult, [pf, gh], [a])
                    k.cp("dve", ki[:], a[:], [a], [ki])
                    k.cp("dve", kf[:], ki[:], [ki], [kf])
                    k.tt("dve", a[:], a[:], kf[:], ALU.subtract, [a, kf], [a])
                    sn = dst[:, :, 24:48]
                    cs = dst[:, :, 0:24]
                    k.ts("dve", m[:], a[:], 0.5, None, ALU.is_ge, None, [a], [m])
                    k.tt("dve", sn, a[:], m[:], ALU.subtract, [a, m], [dst])
                    k.ts("dve", m[:], sn, -0.5, None, ALU.is_lt, None, [dst], [m])
                    k.tt("dve", sn, sn, m[:], ALU.add, [dst, m], [dst])
                    k.ts("dve", cs, sn, 0.25, None, ALU.add, None, [dst], [dst])
                    k.ts("dve", m[:], cs, 0.5, None, ALU.is_ge, None, [dst], [m])
                    k.tt("dve", cs, cs, m[:], ALU.subtract, [dst, m], [dst])
                    k.ts("dve", m[:], cs, -0.5, None, ALU.is_lt, None, [dst], [m])
                    k.tt("dve", cs, cs, m[:], ALU.add, [dst, m], [dst])
                    k.act(dst[:], dst[:], AF.Sin, [dst], [dst], scale=TWO_PI)
                barrier()
            k.es = es2

            def csv(tab, b_):
                small["cs"] = tab
                return tab[:, b_, 0:16], tab[:, b_, 16:24], tab[:, b_, 24:40], tab[:, b_, 40:48]
            NCK = k.sb("NCK", [128, NB, FH], F32)
            carry = k.sb("carry", [128, FH], F32)
            kin = [k.sb(f"kin{i}", [128, 1544], F32) for i in range(2)]
            kn2 = [k.sb(f"kn{i}", [128, 1024], F32) for i in range(2)]
            sqb2 = [k.sb(f"sqb{i}", [128, 768], F32) for i in range(2)]
            knb2 = [k.sb(f"knb{i}", [128, 1024], BF16) for i in range(2)]
            rtmp2 = [k.sb(f"rtmp{i}", [128, 512], F32) for i in range(2)]
            ss2 = [k.sb(f"ssp{i}", [128, 16], F32) for i in range(2)]
            lf2 = [k.sb(f"lfp{i}", [128, 8], F32) for i in range(2)]
            kn, sqb, knb, rtmp = kn2[0], sqb2[0], knb2[0], rtmp2[0]
            lf = k.sb("lf", [128, 8], F32)
            qT = k.sb("qT", [128, FH, 128], BF16)
            pT_sb = [k.sb(f"pTsb{i}", [128, 128], BF16) for i in range(4)]
            rec = [k.sb(f"rec{i}", [128, 128], F32) for i in range(2)]
            cref = k.sb("cref", [128, FH], F32)
            biasall = k.sb("biasall", [128, NB, FH], F32)
            pcnt = {"s": 0, "t": 0, "k": 0, "a": 0}
            qin = kin[0]
            qn, qnb = kn, knb

            def tr_bank():
                pb = PB[0]
                return pb, pb[:].bitcast(BF16).rearrange("p (c t) -> p c t", c=8)

            def kside_block(mode, tb, KT, VV):
                par = tb % 2
                ki_, knp, sqp, knbp, rtp, ssp, lfp = kin[par], kn2[par], sqb2[par], knb2[par], rtmp2[par], ss2[par], lf2[par]
                pb = PB[0] if par == 0 else PB[1]
                pv = pb[:].bitcast(BF16).rearrange("p (c t) -> p c t", c=8)
                rows = slice(tb * 128, (tb + 1) * 128)
                kcol = K_FK if mode == "fox" else K_DK
                vcol = K_FV if mode == "fox" else K_DV
                k.dmaq("sp", ki_[:, 0:768], ksc[rows, kcol:kcol + 768], reads=[t_ksc], writes=[ki_])
                k.dmaq("sp", ki_[:, 768:1536], ksc[rows, vcol:vcol + 768], reads=[t_ksc], writes=[ki_])
                if mode == "fox":
                    k.dmaq("sp", ki_[:, 1536:1542], ksc[rows, K_FL:K_FL + 6], reads=[t_ksc], writes=[ki_])
                yield
                src = ki_[:, 0:768]
                ss = ssp[:, 0:FH]
                k.tt("dve", sqp[:, 0:768], src, src, ALU.mult, [ki_], [sqp])
                k.op("dve", lambda q: q.tensor_reduce(out=ss, in_=sqp[:, 0:768].rearrange("p (h d) -> p h d", h=FH),
                                                      axis=AX.X, op=ALU.add), [sqp], [ssp])
                k.ts("dve", ss, ss, 1.0 / DH, EPS, ALU.mult, ALU.add, [ssp], [ssp])
                if mode == "fox":
                    k.tt("dve", lfp[:, 0:6], ki_[:, 1536:1542], FB, ALU.add, [ki_, gh], [lfp])
                yield
                k.act(ss, ss, AF.Sqrt, [ssp], [ssp])
                if mode == "fox":
                    k.act(lfp[:, 0:6], lfp[:, 0:6], AF.Exp, [lfp], [lfp], scale=-1.0)
                    k.act(lfp[:, 0:6], lfp[:, 0:6], AF.Ln, [lfp], [lfp], bias=1.0)
                yield
                k.op("dve", lambda q: q.reciprocal(out=ss, in_=ss), [ssp], [ssp])
                s3 = src.rearrange("p (h d) -> p h d", h=FH)
                d3 = knp[:, 0:768].rearrange("p (h d) -> p h d", h=FH)
                gain = G_FK if mode == "fox" else G_DK
                k.tt("dve", d3, s3, ss.unsqueeze(2).to_broadcast([128, FH, DH]), ALU.mult, [ki_, ssp], [knp])
                k.tt("dve", d3, d3, gain.unsqueeze(1).to_broadcast([128, FH, DH]), ALU.mult, [knp], [knp])
                if mode == "dsaB":
                    c16, c8, s16, s8 = csv(CSA, tb)
                    rope(knp, d3, FH, 16, c16, s16, rtp)
                k.cp("act", VV[:, tb, :], ki_[:, 768:1536], [ki_], [VV.part(tb)])
                yield
                k.cp("act", knbp[:, 0:768], knp[:, 0:768], [knp], [knbp])
                yield
                for h in range(FH):
                    k.tr(pv[:, h, :], knbp[:, h * 128:(h + 1) * 128], identB[:], [knbp, identB], [pb])
                if mode == "fox":
                    pc = PB[7]
                    k.mm(pc[:, par * 32:par * 32 + 6], tri[:], lfp[:, 0:6], True, True, [tri, lfp], [pc])
                    k.mm(pc[:, par * 32 + 8:par * 32 + 14], onesF[:], lfp[:, 0:6], True, True, [onesF, lfp], [pc])
                yield
                k.cp("dve", KT[:, :, rows], pv[:, 0:FH, :], [pb], [KT.part(tb)])
                if mode == "fox":
                    pc = PB[7]
                    k.tt("dve", NCK[:, tb, :], pc[:, par * 32:par * 32 + 6], carry[:], ALU.add, [pc, carry], [NCK.part(tb)])
                    k.tt("dve", carry[:], pc[:, par * 32 + 8:par * 32 + 14], carry[:], ALU.add, [pc, carry], [carry])

            def kside_pass(mode, KT, VV, IKT):
                if mode == "fox":
                    k.op("pool", lambda q: q.memset(carry[:], 0.0), [], [carry])
                if mode != "dsaA":
                    for tb0 in range(0, NB, 2):
                        gens = [kside_block(mode, tb0, KT, VV), kside_block(mode, tb0 + 1, KT, VV)]
                        live = list(gens)
                        while live:
                            nxt = []
                            for g_ in live:
                                try:
                                    next(g_)
                                    nxt.append(g_)
                                except StopIteration:
                                    pass
                            live = nxt
                    return
                for tb in range(NB):
                    ki_ = kin[pcnt["k"] % 2]
                    pcnt["k"] += 1
                    rows = slice(tb * 128, (tb + 1) * 128)
                    COS16, COS8, SIN16, SIN8 = csv(CSA, tb)
                    k.dmaq("sp", ki_[:, 0:64], ksc[rows, K_IK:K_IK + 64], reads=[t_ksc], writes=[ki_])
                    head_norm(ki_, ki_[:, 0:64], 1, 64, G_IK, kn, kn[:, 0:64], sqb)
                    rope(kn, kn[:, 0:64].rearrange("p (h d) -> p h d", h=1), 1, 8, COS8, SIN8, rtmp)
                    k.cp("dve", kn[:, 64:128], kn[:, 0:64], [kn], [kn])
                    k.cp("act", knb[:, 0:128], kn[:, 0:128], [kn], [knb])
                    pb, pv = tr_bank()
                    k.tr(pv[:, 0, :], knb[:, 0:128], identB[:], [knb, identB], [pb])
                    k.cp("dve", IKT[0][0:64, rows], pv[0:64, 0, :], [pb], [IKT[0].part(tb)])
                    k.cp("dve", IKT[1][64:128, rows], pv[64:128, 0, :], [pb], [IKT[1].part(tb)])

            def attention(i, nheads, nkb, KT, VV, mask_fn, bias_fn, ot_base):
                scale = DH ** -0.5
                for h in range(nheads):
                    pO, pD = ((PB[5], PB[6]), (PB[7], PB[1]))[pcnt["a"] % 2]
                    pcnt["a"] += 1
                    pend = []

                    def pv_step(kb, pt):
                        kr = [KT.part(kb), VV.part(kb)]
                        k.mm(pO[:, 0:128], VV[:, kb, h * 128:(h + 1) * 128], pt[:], kb == 0, kb == nkb - 1, kr + [pt], [pO])
                        k.mm(pD[:, 0:128], onesB[:], pt[:], kb == 0, kb == nkb - 1, [onesB, pt], [pD])
                    for kb in range(nkb):
                        pS = PB[2 + pcnt["s"] % 3]
                        pt = pT_sb[pcnt["s"] % 4]
                        pcnt["s"] += 1
                        mk = mask_fn(kb) if mask_fn else None
                        kr = [KT.part(kb), VV.part(kb)]
                        k.mm(pS[:, 0:128], KT[:, h, kb * 128:(kb + 1) * 128], qT[:, h, :], True, mk is None, kr + [qT], [pS])
                        if mk is not None:
                            k.mm(pS[:, 0:128], identB[:], mk[0], False, True, [identB] + mk[1], [pS])
                        if bias_fn is not None:
                            bap, brd = bias_fn(kb, h)
                            k.act(pt[:], pS[:, 0:128], AF.Exp, [pS] + brd, [pt], scale=scale, bias=bap)
                        else:
                            k.act(pt[:], pS[:, 0:128], AF.Exp, [pS], [pt], scale=scale)
                        pend.append((kb, pt))
                        if len(pend) > 2:
                            pv_step(*pend.pop(0))
                    while pend:
                        pv_step(*pend.pop(0))
                    rc = rec[pcnt["a"] % 2]
                    k.op("dve", lambda q: q.reciprocal(out=rc[:], in_=pD[:, 0:128]), [pD], [rc])
                    k.tt("dve", OT[:, ot_base + h, i * 128:(i + 1) * 128], pO[:, 0:128], rc[:], ALU.mult, [pO, rc],
                         [OT.part((ot_base + h, i))])

            def load_q_heads(i, col, H, gain, do_rope, cs16=None):
                rows = slice(i * 128, (i + 1) * 128)
                k.dmaq("sp", qin[:, 0:H * DH], qsc[rows, col:col + H * DH], reads=[t_qsc], writes=[qin])
                head_norm(qin, qin[:, 0:H * DH], H, DH, gain, qn, qn[:, 0:H * DH], sqb)
                if do_rope:
                    rope(qn, qn[:, 0:H * DH].rearrange("p (h d) -> p h d", h=H), H, 16, cs16[0], cs16[1], rtmp)
                k.cp("act", qnb[:, 0:H * DH], qn[:, 0:H * DH], [qn], [qnb])
                pb, pv = tr_bank()
                for h in range(H):
                    k.tr(pv[:, h, :], qnb[:, h * 128:(h + 1) * 128], identB[:], [qnb, identB], [pb])
                k.cp("dve", qT[:, 0:H, :], pv[:, 0:H, :], [pb], [qT])

            def dump_ot(name, h0, h1):
                if debug and name in dbg:
                    tmpd = kn
                    for h in range(h0, h1):
                        k.cp("dve", tmpd[:, 0:T], OT[:, h, :], [OT.part((h, i_)) for i_ in range(NQ)], [tmpd])
                        k.dmaq("sp", dbg[name][(h - h0) * 128:(h - h0 + 1) * 128, :], tmpd[:, 0:T], reads=[tmpd])

            with ExitStack() as esf:
                k.es = esf
                KT = k.sb("KTf", [128, FH, S], BF16)
                VV = k.sb("VVf", [128, NB, FH * DH], BF16)
                kside_pass("fox", KT, VV, None)
                for i in range(NQ):
                    nkb = 4 * (i + 1)
                    load_q_heads(i, Q_FQ, FH, G_FQ, False)
                    pc = PB[7]
                    for r in range(4):
                        k.mm(pc[:, 16:22], selF[:, r, :], NCK[:, 4 * i + r, :], r == 0, r == 3, [selF, NCK.part(4 * i + r)], [pc])
                    k.cp("dve", cref[:], pc[:, 16:22], [pc], [cref])
                    for kb in range(nkb):
                        k.tt("dve", biasall[:, kb, :], NCK[:, kb, :], cref[:], ALU.subtract, [NCK.part(kb), cref], [biasall])
                    attention(i, FH, nkb, KT, VV,
                              (lambda kb, i=i: ((cmB[:, kb - 4 * i, :], [cmB]) if kb >= 4 * i else None)),
                              (lambda kb, h: (biasall[:, kb, h:h + 1], [biasall])), 0)
                dump_ot("ofox", 0, FH)
                barrier()
            k.es = es2

            with ExitStack() as esa:
                k.es = esa
                IKT0 = k.sb("IKT0", [128, S], BF16)
                IKT1 = k.sb("IKT1", [128, S], BF16)
                SCb = [k.sb(f"SC{i}", [128, S], F32) for i in range(2)]
                WK = k.sb("WK", [128, 512], F32)
                MB = k.sb("MB", [128, S], BF16)
                MBT = k.sb("MBT", [128, NB, 128], BF16)
                mx = k.sb("mx", [128, 8], F32)
                bs = k.sb("bs", [128, 8], F32)
                NIT = 16
                HV = k.sb("HV", [128, NIT], F32)
                pw2 = k.sb("pw2", [128, NIT], F32)
                for it in range(NIT):
                    k.op("pool", lambda q, it=it: q.memset(pw2[:, it:it + 1], 2.0 ** -(it + 1)), [], [pw2])
                rl = [k.sb(f"rl{i}", [128, 512], F32) for i in range(5)]
                iqT = k.sb("iqT", [128, 8, 128], BF16)
                wsc = k.sb("wsc", [128, IH], F32)
                Dg = k.sb("Dg", [128, IH, 128], F32)
                k.op("pool", lambda q: q.memset(IKT0[:], 0.0), [], [IKT0])
                k.op("pool", lambda q: q.memset(IKT1[:], 0.0), [], [IKT1])
                kside_pass("dsaA", None, None, (IKT0, IKT1))
                IK = (IKT0, IKT1)
                icnt = 0
                acnt = 0
                cmTf = cmT[:].rearrange("p r k -> p (r k)")
                for i in range(NQ):
                    nkb = 4 * (i + 1)
                    L = nkb * 128
                    SC = SCb[i % 2]
                    rows = slice(i * 128, (i + 1) * 128)
                    COS16, COS8, SIN16, SIN8 = csv(CSO, i)
                    k.dmaq("sp", qin[:, 0:1024 + 16], qsc[rows, Q_IQ:Q_IQ + 1024 + 16], reads=[t_qsc], writes=[qin])
                    rope(qin, qin[:, 0:1024].rearrange("p (h d) -> p h d", h=IH), IH, 8, COS8, SIN8, rtmp)
                    k.cp("act", qnb[:, 0:1024], qin[:, 0:1024], [qin], [qnb])
                    k.ts("dve", wsc[:], qin[:, 1024:1040], (IH ** -0.5) * (IDD ** -0.5), None, ALU.mult, None, [qin], [wsc])
                    for j in range(IH):
                        k.ts("pool", Dg[:, j, :], identF[:], wsc[:, j:j + 1], 1.0, ALU.mult, ALU.mult, [identF, wsc], [Dg])
                    pb, pv = tr_bank()
                    for m in range(8):
                        k.tr(pv[:, m, :], qnb[:, m * 128:(m + 1) * 128], identB[:], [qnb, identB], [pb])
                    k.cp("dve", iqT[:], pv, [pb], [iqT])
                    nch = L // 512
                    for kc in range(nch):
                        ks = slice(kc * 512, (kc + 1) * 512)
                        pA = PB[5 + acnt % 2]
                        acnt += 1
                        last = (kc == nch - 1)
                        pend = []

                        def diag(jj, rbuf):
                            k.mm(pA[:], Dg[:, jj, :], rbuf[:], jj == 0, (jj == IH - 1) and not last, [Dg, rbuf], [pA])
                        for j in range(IH):
                            m, r = j // 2, j % 2
                            pS = PB[1 + icnt % 4]
                            rb_ = rl[icnt % 5]
                            icnt += 1
                            k.mm(pS[:], iqT[:, m, :], IK[r][:, ks], True, True,
                                 [iqT] + [IK[r].part(kc * 4 + t_) for t_ in range(4)], [pS])
                            k.act(rb_[:], pS[:], AF.Relu, [pS], [rb_])
                            pend.append((j, rb_))
                            if len(pend) > 3:
                                diag(*pend.pop(0))
                        while pend:
                            diag(*pend.pop(0))
                        if last:
                            k.mm(pA[:], identF[:], cmTf, False, True, [identF, cmT], [pA])
                        k.cp("act", SC[:, ks], pA[:], [pA], [SC.part(kc)])
                    scp = [SC.part(c) for c in range(nch)]
                    if i == 0:
                        cur, curp = SC, scp
                        nround = cfg.topk // 8
                        for rnd in range(nround):
                            k.op("dve", lambda q, cur=cur: q.max(out=mx[:], in_=cur[:, 0:L]), curp, [mx])
                            if rnd < nround - 1:
                                k.op("dve", lambda q, cur=cur: q.match_replace(out=WK[:, 0:L], in_to_replace=mx[:],
                                                                              in_values=cur[:, 0:L], imm_value=-1e30),
                                     curp + [mx], [WK])
                                cur, curp = WK, [WK]
                        k.ts("dve", mx[:, 7:8], mx[:, 7:8], -10000.0, None, ALU.max, None, [mx], [mx])
                    else:
                        lo, hi, mid, cntc, ge = (bs[:, c:c + 1] for c in range(5))
                        k.op("dve", lambda q: q.tensor_reduce(out=lo, in_=SC[:, 0:L - 512], axis=AX.X, op=ALU.min), scp, [bs])
                        k.op("dve", lambda q: q.tensor_reduce(out=hi, in_=SC[:, 0:L], axis=AX.X, op=ALU.max), scp, [bs])
                        k.ts("dve", hi, hi, 1e-3, None, ALU.add, None, [bs], [bs])
                        k.tt("dve", hi, hi, lo, ALU.subtract, [bs], [bs])
                        k.ts("dve", HV[:], pw2[:], hi, None, ALU.mult, None, [pw2, bs], [HV])
                        for it in range(NIT):
                            k.tt("dve", mid, lo, HV[:, it:it + 1], ALU.add, [bs, HV], [bs])
                            k.op("dve", lambda q: q.tensor_scalar(out=MB[:, 0:L], in0=SC[:, 0:L], scalar1=mid, scalar2=None,
                                                                  op0=ALU.is_ge, op1=ALU.add, accum_out=cntc),
                                 scp + [bs], [MB, bs])
                            k.ts("dve", ge, cntc, float(cfg.topk) - 0.5, None, ALU.is_ge, None, [bs], [bs])
                            k.stt(lo, ge, HV[:, it:it + 1], lo, ALU.mult, ALU.add, [bs, HV], [bs])
                        k.cp("dve", mx[:, 7:8], lo, [bs], [mx])
                    k.ts("dve", MB[:, 0:L], SC[:, 0:L], mx[:, 7:8], NEG, ALU.is_lt, ALU.mult, scp + [mx], [MB])
                    for kb0 in range(0, nkb, 4):
                        pb, pv = tr_bank()
                        for c in range(4):
                            kb = kb0 + c
                            k.tr(pv[:, c, :], MB[:, kb * 128:(kb + 1) * 128], identB[:], [MB, identB], [pb])
                        k.cp("act", MBT[:, kb0:kb0 + 4, :], pv[:, 0:4, :], [pb], [MBT])
                    k.dmaq("pool", mbsc[i, :, 0:L], MBT[:, 0:nkb, :].rearrange("p a b -> p (a b)"), reads=[MBT], writes=[t_mbsc])
                barrier()
            k.es = es2

            with ExitStack() as esb:
                k.es = esb
                KT = k.sb("KTd", [128, FH, S], BF16)
                VV = k.sb("VVd", [128, NB, FH * DH], BF16)
                MBI = [k.sb(f"MBI{i}", [128, NB, 128], BF16) for i in range(1)]
                kside_pass("dsaB", KT, VV, None)
                for i in range(NQ):
                    nkb = 4 * (i + 1)
                    L = nkb * 128
                    rows = slice(i * 128, (i + 1) * 128)
                    mbi = MBI[0]
                    k.dmaq("sp", mbi[:, 0:nkb, :].rearrange("p a b -> p (a b)"), mbsc[i, :, 0:L], reads=[t_mbsc], writes=[mbi])
                    COS16, COS8, SIN16, SIN8 = csv(CSO, i)
                    load_q_heads(i, Q_DQ, FH, G_DQ, True, (COS16, SIN16))
                    attention(i, FH, nkb, KT, VV, (lambda kb, mbi=mbi: (mbi[:, kb, :], [mbi])), None, FH)
                dump_ot("odsa", FH, 2 * FH)
                barrier()
                for mb_ in range(NMEM // 128):
                    ki_ = kin[1]
                    rows = slice(mb_ * 128, (mb_ + 1) * 128)
                    k.dmaq("sp", ki_[:, 0:1024], msc[rows, :], reads=[t_msc], writes=[ki_])
                    head_norm(ki_, ki_[:, 0:512], CH, DH, G_CK, kn, kn[:, 0:512], sqb)
                    k.cp("act", knb[:, 0:512], kn[:, 0:512], [kn], [knb])
                    pb, pv = tr_bank()
                    for h in range(CH):
                        k.tr(pv[:, h, :], knb[:, h * 128:(h + 1) * 128], identB[:], [knb, identB], [pb])
                    k.cp("dve", KT[:, 0:CH, rows], pv[:, 0:CH, :], [pb], [KT.part(mb_)])
                    k.cp("pool", VV[:, mb_, 0:512], ki_[:, 512:1024], [ki_], [VV.part(mb_)])
                for i in range(NQ):
                    load_q_heads(i, Q_CQ, CH, G_CQ, False)
                    attention(i, CH, NMEM // 128, KT, VV, None, None, 2 * FH)
                dump_ot("ocross", 2 * FH, 2 * FH + CH)
                barrier()
            k.es = es2
            barrier()
        k.es = es

        with ExitStack() as es3:
            k.es = es3
            small["ss"] = k.sb("ss3", [128, 16], F32)
            MT = k.sb("MT", [128, KC, T], BF16)
            stg = [k.sb(f"stg{i}", [128, 1024], F32) for i in range(2)]
            sc_ = 0
            pcn = 0
            with ExitStack() as es3a:
                k.es = es3a
                WB = k.sb("WB", [128, 2 * FH + CH, D], BF16)
                for bi, (wsrc, nh) in enumerate(((wbf_d, FH), (wbd_d, FH), (wbc_d, CH))):
                    for h in range(nh):
                        hh = (0, FH, 2 * FH)[bi] + h
                        for hf in range(2):
                            st = stg[sc_ % 2]
                            sc_ += 1
                            k.dmaq("sp", st[:], wsrc[h * 128:(h + 1) * 128, hf * 1024:(hf + 1) * 1024], writes=[st])
                            k.cp("dve" if sc_ % 2 else "act", WB[:, hh, hf * 1024:(hf + 1) * 1024], st[:], [st], [WB.part(hh)])
                gt = k.sb("gt", [128, 3 * D], F32)
                mg = k.sb("mg", [128, D], F32)
                mgt = k.sb("mgt", [128, 512], F32)
                mgb = k.sb("mgb", [128, D], BF16)
                for i in range(NQ):
                    rows = slice(i * 128, (i + 1) * 128)
                    k.dmaq("sp", gt[:], qsc[rows, Q_GT:Q_GT + 3 * D], reads=[t_qsc], writes=[gt])
                    k.act(gt[:], gt[:], AF.Sigmoid, [gt], [gt])
                    for bi, (nh, base) in enumerate(((FH, 0), (FH, FH), (CH, 2 * FH))):
                        for dc in range(4):
                            ds_ = slice(dc * 512, (dc + 1) * 512)
                            pb = PB[2 + pcn % 3]
                            pcn += 1
                            for h in range(nh):
                                k.mm(pb[:], OT[:, base + h, rows], WB[:, base + h, ds_], h == 0, h == nh - 1,
                                     [OT.part((base + h, i)), WB.part(base + h)], [pb])
                            gsl = gt[:, bi * D + dc * 512: bi * D + (dc + 1) * 512]
                            if bi == 0:
                                k.tt("dve", mg[:, ds_], pb[:], gsl, ALU.mult, [pb, gt], [mg])
                            else:
                                k.tt("dve", mgt[:], pb[:], gsl, ALU.mult, [pb, gt], [mgt])
                                k.tt("dve", mg[:, ds_], mg[:, ds_], mgt[:], ALU.add, [mg, mgt], [mg])
                    if debug and "mg" in dbg:
                        k.dmaq("sp", dbg["mg"][rows, :], mg[:], reads=[mg])
                    k.cp("act", mgb[:], mg[:], [mg], [mgb])
                    for half in range(2):
                        pb = PB[pcn % 2]
                        pcn += 1
                        pv = pb[:].bitcast(BF16).rearrange("p (c t) -> p c t", c=8)
                        for c in range(8):
                            kc = half * 8 + c
                            k.tr(pv[:, c, :], mgb[:, kc * 128:(kc + 1) * 128], identB[:], [mgb, identB], [pb])
                        k.cp("dve", MT[:, half * 8:(half + 1) * 8, rows], pv, [pb], [MT.part(i)])
                barrier()
            k.es = es3
            OT_guard.__exit__(None, None, None)
            esR = ExitStack()
            H2 = Buf(esR.enter_context(nc.sbuf_tensor("H2", [128, NQ, D], BF16, side="right")))
            CO = Buf(esR.enter_context(nc.sbuf_tensor("CO", [128, NQ, E], F32, side="right")))
            AM = Buf(esR.enter_context(nc.sbuf_tensor("AM", [128, NQ, E], F32, side="right")))
            POS = Buf(esR.enter_context(nc.sbuf_tensor("POS", [128, NQ, E], F32, side="right")))
            with ExitStack() as es3b:
                k.es = es3b
                WO = k.sb("WO", [128, KC, D], BF16)
                WR = k.sb("WR", [128, KC, NR], F32)
                for kc in range(KC):
                    for hf in range(2):
                        st = stg[sc_ % 2]
                        sc_ += 1
                        k.dmaq("sp", st[:], wout_d[kc * 128:(kc + 1) * 128, hf * 1024:(hf + 1) * 1024], writes=[st])
                        k.cp("dve" if sc_ % 2 else "act", WO[:, kc, hf * 1024:(hf + 1) * 1024], st[:], [st], [WO.part(kc)])
                wrs = k.sb("wrs", [128, KC, NR], F32)
                k.dmaq("sp", wrs[:], wr_d.rearrange("(kc p) c -> p kc c", p=128), writes=[wrs])
                for kc in range(KC):
                    k.ts("dve", WR[:, kc, :], wrs[:, kc, :], gcols[:, gFf + kc:gFf + kc + 1], None, ALU.mult, None,
                         [wrs, gcols], [WR])
                triS = k.sb("triS", [128, 128], F32)
                carryE = k.sb("carryE", [128, E], F32)
                k.op("pool", lambda q: q.memset(triS[:], 1.0), [], [triS])
                k.op("pool", lambda q: q.affine_select(out=triS[:], in_=triS[:], pattern=[[1, 128]], compare_op=ALU.is_gt,
                                                       fill=0.0, base=0, channel_multiplier=-1), [triS], [triS])
                k.op("pool", lambda q: q.memset(carryE[:], 0.0), [], [carryE])
                xo_sb = k.sb("xo_sb", [128, D], F32)
                x1 = k.sb("x1", [128, D], F32)
                h2f = k.sb("h2f", [128, D], F32)
                h2fT = k.sb("h2fT", [128, KC, 128], F32)
                ss1 = k.sb("ss1b", [128, 1], F32)
                lg = k.sb("lg", [128, NR], F32)
                rt = k.sb("rt", [128, 8 * E + 64], F32)
                for i in range(NQ):
                    rows = slice(i * 128, (i + 1) * 128)
                    k.dmaq("sp", xo_sb[:], xo[rows, :], writes=[xo_sb])
                    for dc in range(4):
                        ds_ = slice(dc * 512, (dc + 1) * 512)
                        pb = PB[2 + pcn % 3]
                        pcn += 1
                        for kc in range(KC):
                            k.mm(pb[:], MT[:, kc, rows], WO[:, kc, ds_], kc == 0, kc == KC - 1, [MT.part(i), WO.part(kc)], [pb])
                        k.tt("dve", x1[:, ds_], pb[:], xo_sb[:, ds_], ALU.add, [pb, xo_sb], [x1])
                    k.dmaq("pool", x1sc[rows, :], x1[:], reads=[x1], writes=[t_x1])
                    if debug and "x1" in dbg:
                        k.dmaq("sp", dbg["x1"][rows, :], x1[:], reads=[x1])
                    k.act(h2f[:], x1[:], AF.Square, [x1], [h2f, ss1], accum_out=ss1[:])
                    k.ts("dve", ss1[:], ss1[:], 1.0 / D, EPS, ALU.mult, ALU.add, [ss1], [ss1])
                    k.act(ss1[:], ss1[:], AF.Sqrt, [ss1], [ss1])
                    k.op("dve", lambda q: q.reciprocal(out=ss1[:], in_=ss1[:]), [ss1], [ss1])
                    k.ts("dve", h2f[:], x1[:], ss1[:, 0:1], None, ALU.mult, None, [x1, ss1], [h2f])
                    for q4 in range(4):
                        pb = PB[5 + pcn % 2]
                        pcn += 1
                        for c in range(4):
                            kc = q4 * 4 + c
                            k.tr(pb[:, c * 128:(c + 1) * 128], h2f[:, kc * 128:(kc + 1) * 128], identF[:], [h2f, identF], [pb])
                        k.cp("act", h2fT[:, q4 * 4:(q4 + 1) * 4, :], pb[:].rearrange("p (c t) -> p c t", c=4), [pb], [h2fT])
                    k.cp("pool", H2[:, i, :], h2f[:], [h2f], [H2.part(i)])
                    pb = PB[7]
                    for kc in range(KC):
                        k.mm(pb[:, 0:NR], h2fT[:, kc, :], WR[:, kc, :], kc == 0, kc == KC - 1, [h2fT, WR], [pb])
                    k.tt("dve", lg[:], pb[:, 0:NR], RB, ALU.add, [pb, gh], [lg])
                    gmax = rt[:, 0:1]
                    ngmax = rt[:, 1:2]
                    gsum = rt[:, 2:3]
                    gex = rt[:, 4:4 + G]
                    ohg = rt[:, 16:16 + G]
                    pen = rt[:, 32:32 + G]
                    o0 = 64
                    em = rt[:, o0:o0 + E]
                    oh1 = rt[:, o0 + E:o0 + 2 * E]
                    oh2 = rt[:, o0 + 2 * E:o0 + 3 * E]
                    em2 = rt[:, o0 + 3 * E:o0 + 4 * E]
                    m1 = rt[:, 40:41]
                    m2 = rt[:, 41:42]
                    dd_ = rt[:, 42:43]
                    w1 = rt[:, 43:44]
                    w2 = rt[:, 44:45]
                    R = [rt, lg]
                    k.op("dve", lambda q: q.tensor_reduce(out=gmax, in_=lg[:, 0:G], axis=AX.X, op=ALU.max), [lg], [rt])
                    k.ts("dve", ngmax, gmax, -1.0, None, ALU.mult, None, [rt], [rt])
                    k.act(gex, lg[:, 0:G], AF.Exp, R, [rt], bias=ngmax, accum_out=gsum)
                    k.op("dve", lambda q: q.reciprocal(out=gsum, in_=gsum), [rt], [rt])
                    k.ts("dve", ohg, lg[:, 0:G], gmax, None, ALU.is_equal, None, R, [rt])
                    k.ts("dve", pen, ohg, -1.0, 1e9, ALU.add, ALU.mult, [rt], [rt])
                    k.tt("dve", em.rearrange("p (g e) -> p g e", g=G), lg[:, G:NR].rearrange("p (g e) -> p g e", g=G),
                         pen.unsqueeze(2).to_broadcast([128, G, EPG]), ALU.add, R, [rt])
                    k.op("dve", lambda q: q.tensor_reduce(out=m1, in_=em, axis=AX.X, op=ALU.max), [rt], [rt])
                    k.ts("dve", oh1, em, m1, None, ALU.is_equal, None, [rt], [rt])
                    k.stt(em2, oh1, -1e9, em, ALU.mult, ALU.add, [rt], [rt])
                    k.op("dve", lambda q: q.tensor_reduce(out=m2, in_=em2, axis=AX.X, op=ALU.max), [rt], [rt])
                    k.ts("dve", oh2, em2, m2, None, ALU.is_equal, None, [rt], [rt])
                    k.tt("dve", AM[:, i, :], oh1, oh2, ALU.add, [rt], [AM.part(i)])
                    pcs = PB[6]
                    k.mm(pcs[:, 0:E], triS[:], AM[:, i, :], True, True, [triS, AM.part(i)], [pcs])
                    k.tt("dve", POS[:, i, :], pcs[:, 0:E], carryE[:], ALU.add, [pcs, carryE], [POS.part(i)])
                    k.mm(pcs[:, 64:64 + E], onesF[:], AM[:, i, :], True, True, [onesF, AM.part(i)], [pcs])
                    k.tt("dve", carryE[:], pcs[:, 64:64 + E], carryE[:], ALU.add, [pcs, carryE], [carryE])
                    k.tt("dve", dd_, m2, m1, ALU.subtract, [rt], [rt])
                    k.act(dd_, dd_, AF.Exp, [rt], [rt])
                    k.ts("dve", w1, dd_, 1.0, None, ALU.add, None, [rt], [rt])
                    k.op("dve", lambda q: q.reciprocal(out=w1, in_=w1), [rt], [rt])
                    k.tt("dve", w2, dd_, w1, ALU.mult, [rt], [rt])
                    k.tt("dve", w1, w1, gsum, ALU.mult, [rt], [rt])
                    k.tt("dve", w2, w2, gsum, ALU.mult, [rt], [rt])
                    k.ts("dve", oh1, oh1, w1, None, ALU.mult, None, [rt], [rt])
                    k.stt(CO[:, i, :], oh2, w2, oh1, ALU.mult, ALU.add, [rt], [CO.part(i)])
                    if debug and "co" in dbg:
                        k.dmaq("sp", dbg["co"][rows, :], CO[:, i, :], reads=[CO.part(i)])
                barrier()
            k.es = es3
            barrier()
        k.es = es

        CAP = 128
        EG = 2
        with ExitStack() as es4:
            k.es = es4
            Y = k.sb("Y", [128, NQ, D], F32)
            iotaC = k.sb("iotaC", [128, CAP], F32)
            k.op("pool", lambda q: q.iota(iotaC[:], pattern=[[1, CAP]], base=0, channel_multiplier=0,
                                          allow_small_or_imprecise_dtypes=True), [], [iotaC])
            st4 = [k.sb(f"st4_{i}", [128, 2, 1024], F32) for i in range(4)]
            wb4 = [k.sb(f"wb4_{i}", [128, 2, 1024], BF16) for i in range(6)]
            SG = k.sb("SG", [128, NQ, CAP], BF16)
            SW = k.sb("SW", [128, NQ, CAP], BF16)
            XS = k.sb("XS", [128, KC, CAP], BF16)
            XSs = k.sb("XSs", [128, D], BF16)
            sg = [k.sb(f"sg{i}", [128, 512], F32) for i in range(2)]
            HD = k.sb("HD", [128, FF], BF16)
            HT = k.sb("HT", [128, FF // 128, CAP], BF16)
            YE = [k.sb(f"YE{g}", [128, D], BF16) for g in range(EG)]
            ST = [k.sb(f"ST{g}", [128, T], BF16) for g in range(EG)]
            c4 = {"s": 0, "x": 0}
            firstg = True

            def stream(src_rows_ap, kind, gofs):
                st = st4[c4["s"] % 4]
                wb = wb4[c4["s"] % 6]
                n0 = c4["s"]
                c4["s"] += 1
                k.dmaq("sp", st[:], src_rows_ap, writes=[st])
                for c in range(2):
                    for hf in range(2):
                        eng = ("act", "dve")[(2 * c + hf + n0) % 2]
                        hs = slice(hf * 512, (hf + 1) * 512)
                        if kind == "gu":
                            scast(eng, wb[:, c, hs], st[:, c, hs], gcols[:, gofs + c:gofs + c + 1], [st, gcols], [wb.part(2 * c + hf)])
                        else:
                            k.cp(eng, wb[:, c, hs], st[:, c, hs], [st], [wb.part(2 * c + hf)])
                return wb

            SW2 = [SW, k.sb("SWb", [128, NQ, CAP], BF16)]

            def prep_A(e):
                sw = SW2[e % 2]
                for i in range(NQ):
                    k.ts("dve", SG[:, i, :], iotaC[:], POS[:, i, e:e + 1], AM[:, i, e:e + 1], ALU.is_equal, ALU.mult,
                         [iotaC, POS.part(i), AM.part(i)], [SG.part(i)])
                    k.ts("pool", sw[:, i, :], iotaC[:], POS[:, i, e:e + 1], CO[:, i, e:e + 1], ALU.is_equal, ALU.mult,
                         [iotaC, POS.part(i), CO.part(i)], [sw.part(i)])

            def gather_dc(dc):
                pb = PB[4 + dc % 2]
                for i in range(NQ):
                    k.mm(pb[:], SG[:, i, :], H2[:, i, dc * 512:(dc + 1) * 512], i == 0, i == NQ - 1,
                         [H2.part(i), SG.part(i)], [pb])
                k.cp("act" if dc % 2 else "dve", XSs[:, dc * 512:(dc + 1) * 512], pb[:], [pb], [XSs])

            def gather_tr(half):
                pb = PB[6 + half]
                pv = pb[:].bitcast(BF16).rearrange("p (c t) -> p c t", c=8)
                for c in range(8):
                    kc = half * 8 + c
                    k.tr(pv[:, c, :], XSs[:, kc * 128:(kc + 1) * 128], identB[:], [XSs, identB], [pb])
                k.cp("dve" if half else "act", XS[:, half * 8:(half + 1) * 8, :], pv, [pb], [XS.part(half * 2), XS.part(half * 2 + 1)])

            sc_state = {"first": True}

            def scatter_tile(i, dc):
                ds_ = slice(dc * 512, (dc + 1) * 512)
                pb = PB[4 + c4["x"] % 2]
                c4["x"] += 1
                for gg in range(EG):
                    k.mm(pb[:], ST[gg][:, i * 128:(i + 1) * 128], YE[gg][:, ds_], gg == 0, gg == EG - 1,
                         [ST[gg], YE[gg]], [pb])
                if sc_state["first"]:
                    k.cp("dve", Y[:, i, ds_], pb[:], [pb], [Y.part((i, dc))])
                else:
                    k.tt("dve", Y[:, i, ds_], pb[:], Y[:, i, ds_], ALU.add, [pb, Y.part((i, dc))], [Y.part((i, dc))])

            prep_A(0)
            for dc in range(4):
                gather_dc(dc)
            for half in range(2):
                gather_tr(half)
            for e in range(E):
                g = e % EG
                pend_sc = []
                if g == 0 and e > 0:
                    pend_sc = [(i, dc) for i in range(NQ) for dc in range(4)]
                nsl = 2 * (KC // 2)
                per = -(-len(pend_sc) // nsl) if pend_sc else 0
                for (src, base) in ((wg_d, 0), (wu_d, 2)):
                    sv = src[e].rearrange("(kc p) f -> p kc f", p=128)
                    for k2 in range(KC // 2):
                        wb = stream(sv[:, k2 * 2:(k2 + 1) * 2, :], "gu", gFf + k2 * 2)
                        for c in range(2):
                            kc = k2 * 2 + c
                            for fh in range(2):
                                k.mm(PB[base + fh][:], XS[:, kc, :], wb[:, c, fh * 512:(fh + 1) * 512], kc == 0, kc == KC - 1,
                                     [XS.part(kc // 4), wb.part(2 * c + fh)], [PB[base + fh]])
                        for _ in range(per):
                            if pend_sc:
                                scatter_tile(*pend_sc.pop(0))
                while pend_sc:
                    scatter_tile(*pend_sc.pop(0))
                if g == 0 and e > 0:
                    sc_state["first"] = False
                for fh in range(2):
                    sgb = sg[fh]
                    k.act(sgb[:], PB[fh][:], AF.Silu, [PB[fh]], [sgb])
                    k.tt("dve", HD[:, fh * 512:(fh + 1) * 512], sgb[:], PB[2 + fh][:], ALU.mult, [sgb, PB[2 + fh]], [HD])
                pb = PB[6]
                pv = pb[:].bitcast(BF16).rearrange("p (c t) -> p c t", c=8)
                for c in range(FF // 128):
                    k.tr(pv[:, c, :], HD[:, c * 128:(c + 1) * 128], identB[:], [HD, identB], [pb])
                k.cp("act", HT[:], pv, [pb], [HT])
                dv_ = wd_d[e].rearrange("(fc p) d -> p fc d", p=128)
                for fc in range(FF // 128):
                    st = st4[c4["s"] % 4]
                    wb = wb4[c4["s"] % 6]
                    n0 = c4["s"]
                    c4["s"] += 1
                    sflat = st[:].rearrange("p a b -> p (a b)")
                    wflat = wb[:].rearrange("p a b -> p (a b)")
                    k.dmaq("sp", sflat, dv_[:, fc, :], writes=[st])
                    for c in range(4):
                        eng = ("act", "dve")[(c + n0) % 2]
                        k.cp(eng, wflat[:, c * 512:(c + 1) * 512], sflat[:, c * 512:(c + 1) * 512], [st], [wb.part(c)])
                    for dc in range(4):
                        k.mm(PB[dc][:], HT[:, fc, :], wflat[:, dc * 512:(dc + 1) * 512], fc == 0, fc == FF // 128 - 1,
                             [HT, wb.part(dc)], [PB[dc]])
                    if e + 1 < E:
                        if fc == 0:
                            prep_A(e + 1)
                        elif 1 <= fc <= 4:
                            gather_dc(fc - 1)
                        elif fc in (5, 6):
                            gather_tr(fc - 5)
                for dc in range(4):
                    k.cp("act" if dc % 2 else "dve", YE[g][:, dc * 512:(dc + 1) * 512], PB[dc][:], [PB[dc]], [YE[g]])
                sw = SW2[e % 2]
                pb = PB[7]
                pv = pb[:].bitcast(BF16).rearrange("p (c t) -> p c t", c=8)
                for i0 in range(0, NQ, 8):
                    n_ = min(8, NQ - i0)
                    for c in range(n_):
                        k.tr(pv[:, c, :], sw[:, i0 + c, :], identB[:], [sw.part(i0 + c), identB], [pb])
                    k.cp("dve", ST[g][:, i0 * 128:(i0 + n_) * 128].rearrange("p (c t) -> p c t", c=n_), pv[:, 0:n_, :], [pb], [ST[g]])
            for i in range(NQ):
                for dc in range(4):
                    scatter_tile(i, dc)
            for tb in range(NQ):
                rows = slice(tb * 128, (tb + 1) * 128)
                xb_ = st4[tb % 4]
                xv = xb_[:].rearrange("p a b -> p (a b)")
                k.dmaq("sp", xv, x1sc[rows, :], reads=[t_x1], writes=[xb_])
                k.tt("dve", xv, xv, Y[:, tb, :], ALU.add, [xb_] + [Y.part((tb, dc)) for dc in range(4)], [xb_])
                k.dmaq("sp", out_d[rows, :], xv, reads=[xb_])
            e_sp = k.engs["sp"]
            for qn in k.dma:
                for key, val in k.dma[qn][0]:
                    if val > 0:
                        e_sp.q.wait_ge(k.sems[key], val)
            barrier()
        k.es = es
        esR.close()
        print("instructions:", k.nins, "sems:", k.nsem)
    return nc


def host_inputs(cfg, core, x, mem, positions, attn_norm_g, mem_norm_g, w_in, fox_forget_b,
                fox_q_norm_g, fox_k_norm_g, dsa_q_norm_g, dsa_k_norm_g, idx_k_norm_g,
                cross_q_norm_g, cross_k_norm_g, w_mem_kv, w_branch_fox, w_branch_dsa,
                w_branch_cross, w_out, ffn_norm_g, router_group_w, router_group_b,
                router_expert_w, router_expert_b, expert_w_gate, expert_w_up, expert_w_down, shared):
    S, NQ = cfg.S, cfg.NQ
    b, j = core // 4, core % 4
    f32 = np.float32
    own = np.concatenate([np.arange((4 * i + j) * 128, (4 * i + j + 1) * 128) for i in range(NQ)])
    if "wk" not in shared:
        W = np.asarray(w_in[0], f32)
        o = np.cumsum([0, 768, 768, 768, 6, 768, 768, 768, 1024, 16, 64, 512, 3 * D])
        seg = {n: W[:, o[i]:o[i + 1]] for i, n in enumerate(["fq", "fk", "fv", "fl", "dq", "dk", "dv", "iq", "iw", "ik", "cq", "gt"])}
        shared["wk"] = np.ascontiguousarray(np.concatenate([seg["fk"], seg["dk"], seg["fv"], seg["dv"], seg["ik"], seg["fl"]], axis=1))
        shared["wq"] = np.ascontiguousarray(np.concatenate([seg["fq"], seg["dq"], seg["cq"], seg["iq"], seg["iw"], seg["gt"]], axis=1))
        shared["wr"] = np.ascontiguousarray(np.concatenate([np.asarray(router_group_w[0], f32), np.asarray(router_expert_w[0], f32)], axis=1))
        gc = [np.asarray(g[0], f32).reshape(KC, 128).T for g in (attn_norm_g, mem_norm_g, ffn_norm_g)]
        shared["gcols"] = np.ascontiguousarray(np.concatenate(gc, axis=1))
        half16 = np.power(np.float32(500000.0), -np.arange(16, dtype=f32) * 2.0 / 32).astype(f32)
        half8 = np.power(np.float32(500000.0), -np.arange(8, dtype=f32) * 2.0 / 16).astype(f32)
        ifr = (np.concatenate([half16, half8]).astype(np.float64) / (2 * np.pi)).astype(f32)
        shared["gh"] = np.concatenate([np.asarray(v[0], f32).reshape(-1) for v in
                                       (fox_q_norm_g, fox_k_norm_g, dsa_q_norm_g, dsa_k_norm_g, cross_q_norm_g,
                                        cross_k_norm_g, idx_k_norm_g, fox_forget_b, router_group_b, router_expert_b)] + [ifr]).astype(f32)
        shared["ident"] = np.eye(128, dtype=f32)
        for nm, arr in (("wmem", w_mem_kv), ("wbf", w_branch_fox), ("wbd", w_branch_dsa), ("wbc", w_branch_cross),
                        ("wout", w_out), ("wg", expert_w_gate), ("wu", expert_w_up), ("wd", expert_w_down)):
            shared[nm] = np.ascontiguousarray(np.asarray(arr[0], f32))
    kk = np.arange(128)[:, None]
    qq = np.arange(128)[None, :]
    cm = np.zeros((128, 4, 128), f32)
    cmT = np.zeros((128, 4, 128), f32)
    sel = np.zeros((128, 4, 128), f32)
    for r in range(4):
        if r == j:
            cm[:, r, :] = np.where(kk <= qq, 0.0, NEG)
            cmT[:, r, :] = np.where(qq >= kk, 0.0, NEG).T if False else np.where(kk >= qq, 0.0, NEG)
            sel[64, r, :] = 1.0
        elif r > j:
            cm[:, r, :] = NEG
            cmT[:, r, :] = NEG
    m = dict(
        xb=np.ascontiguousarray(np.asarray(x[b], f32)),
        xo=np.ascontiguousarray(np.asarray(x[b], f32)[own]),
        memb=np.ascontiguousarray(np.asarray(mem[b], f32)),
        posb2=np.ascontiguousarray(np.asarray(positions[b], np.int32).reshape(cfg.NB, 128).T),
        poso2=np.ascontiguousarray(np.asarray(positions[b], np.int32)[own].reshape(NQ, 128).T),
        cm=cm, cmT=cmT, sel=sel,
    )
    for nm in ("wk", "wq", "wr", "gcols", "gh", "ident", "wmem", "wbf", "wbd", "wbc", "wout", "wg", "wu", "wd"):
        m[nm] = shared[nm]
    return m, own


_NC_CACHE = {}


def run(cfg, inputs, debug=False, ncores=8):
    key = (cfg.S, cfg.G, cfg.EPG, bool(debug))
    if key not in _NC_CACHE:
        _NC_CACHE[key] = build_nc(cfg, debug)
    nc = _NC_CACHE[key]
    shared = {}
    maps, owns = [], []
    for c in range(ncores):
        m, own = host_inputs(cfg, c, shared=shared, **inputs)
        maps.append(m)
        owns.append(own)
    res = run_bass_kernel_spmd(nc, maps, core_ids=list(range(ncores)))
    B = inputs["x"].shape[0]
    out = np.zeros((B, cfg.S, D), np.float32)
    for c in range(ncores):
        out[c // 4, owns[c], :] = res.results[c]["out"]
    return out, res


def kernel(**inputs):
    cfg = Cfg()
    out, _ = run(cfg, inputs)
    return out
```
